# Optimizing a Trainium2 kernel written in Bass

```python
import jax
import jax.numpy as jnp
from jax import lax
import numpy as np

D_MODEL = 1024
BATCH = 4
SEQ = 4096
DEPTH = 1

GRID_W = 64
CTX_LEN = 256
D_CONV = 1024
CONV_WIDTH = 31
D_RNN = 1024
RNN_HEADS = 4
RNN_BLOCK = D_RNN // RNN_HEADS
SHORT_CONV = 4
LRU_C = 8.0
N_EXPERTS = 64
TOP_K = 8
N_GROUPS = 8
TOPK_GROUPS = 4
D_EXPERT = 256
D_SHARED = 256
ROUTED_SCALE = 2.5
EPS = 1e-6
N_MOD = 6 * D_MODEL
OFF_GLU = 0
OFF_RNN_X = 2 * D_CONV
OFF_RNN_G = OFF_RNN_X + D_RNN
OFF_MERGE = OFF_RNN_G + D_RNN
N_IN = OFF_MERGE + 2 * D_MODEL

kernel_name = 'hybrid_conv_rglru_moe_dit_block'


def rms_norm(x, g):
    x32 = x.astype(jnp.float32)
    y = x32 * lax.rsqrt(jnp.mean(x32 * x32, axis=-1, keepdims=True) + EPS)
    return (y * g.astype(jnp.float32)).astype(x.dtype)


def layer_norm(x, g, b):
    x32 = x.astype(jnp.float32)
    xc = x32 - jnp.mean(x32, axis=-1, keepdims=True)
    y = xc * lax.rsqrt(jnp.mean(xc * xc, axis=-1, keepdims=True) + EPS)
    return (y * g.astype(jnp.float32) + b.astype(jnp.float32)).astype(x.dtype)


def modulate(x, shift, scale):
    return x * (1.0 + scale[:, None, :]) + shift[:, None, :]


def depthwise_conv(u, w, b, pad):
    out = lax.conv_general_dilated(u, w[:, None, :].astype(u.dtype), window_strides=(1,), padding=[pad], dimension_numbers=('NWC', 'WIO', 'NWC'), feature_group_count=u.shape[-1])
    return out + b.astype(u.dtype)


def conformer_conv(glu_in, p, rows):
    a, g = jnp.split(glu_in, 2, axis=-1)
    u = a * jax.nn.sigmoid(g)
    half = CONV_WIDTH // 2
    if rows is None:
        v = depthwise_conv(u, p['w_dw'], p['b_dw'], (half, half))
    else:
        bsz, length, ch = u.shape
        v = depthwise_conv(u.reshape(bsz * rows, GRID_W, ch), p['w_dw'], p['b_dw'], (half, half)).reshape(bsz, length, ch)
    v = jax.nn.silu(layer_norm(v, p['ln_conv_g'], p['ln_conv_b']))
    return v @ p['w_conv_out']


def _lin_combine(e1, e2):
    a1, b1 = e1
    a2, b2 = e2
    return a1 * a2, a2 * b1 + b2


def rglru_direction(u, p, d, h0, reverse):
    pad = (0, SHORT_CONV - 1) if reverse else (SHORT_CONV - 1, 0)
    v = depthwise_conv(u, p['w_sc'][d], p['b_sc'][d], pad)
    vb = v.reshape(v.shape[:-1] + (RNN_HEADS, RNN_BLOCK))
    r = jax.nn.sigmoid(jnp.einsum('blhi,hij->blhj', vb, p['w_rg_a'][d]).reshape(v.shape) + p['b_rg_a'][d])
    i = jax.nn.sigmoid(jnp.einsum('blhi,hij->blhj', vb, p['w_rg_x'][d]).reshape(v.shape) + p['b_rg_x'][d])
    log_a = LRU_C * r.astype(jnp.float32) * jax.nn.log_sigmoid(p['lru_lambda'][d].astype(jnp.float32))
    a = jnp.exp(log_a)
    b = jnp.sqrt(-jnp.expm1(2.0 * log_a)) * (i * v).astype(jnp.float32)
    a_cum, b_cum = lax.associative_scan(_lin_combine, (a, b), reverse=reverse, axis=1)
    return a_cum * h0[:, None, :] + b_cum


def rglru_bidir(u, p, h0_f, h0_b):
    h_f = rglru_direction(u, p, 0, h0_f, False)
    h_b = rglru_direction(u, p, 1, h0_b, True)
    return h_f, h_b


def mixer(h, p, rows, h0_f, h0_b):
    proj = h @ p['w_in']
    y_a = conformer_conv(proj[..., OFF_GLU:OFF_RNN_X], p, rows)
    h_f, h_b = rglru_bidir(proj[..., OFF_RNN_X:OFF_RNN_G], p, h0_f, h0_b)
    gate_branch = jax.nn.gelu(proj[..., OFF_RNN_G:OFF_MERGE])
    y_b = (gate_branch * (h_f + h_b).astype(h.dtype)) @ p['w_rnn_out']
    g_a, g_b = jnp.split(jax.nn.sigmoid(proj[..., OFF_MERGE:N_IN]), 2, axis=-1)
    out = (g_a * y_a + g_b * y_b) @ p['w_out']
    return out, h_f, h_b


def swiglu(t, wg, wu, wd):
    return (jax.nn.silu(t @ wg) * (t @ wu)) @ wd


def moe(h, p):
    bsz, length, dim = h.shape
    t = h.reshape(-1, dim)
    scores = jax.nn.sigmoid((t @ p['w_router']).astype(jnp.float32))
    sel = scores + p['router_bias'].astype(jnp.float32)
    grouped = sel.reshape(-1, N_GROUPS, N_EXPERTS // N_GROUPS)
    group_score = jnp.sum(lax.top_k(grouped, 2)[0], axis=-1)
    _, top_groups = lax.top_k(group_score, TOPK_GROUPS)
    group_mask = jnp.sum(jax.nn.one_hot(top_groups, N_GROUPS, dtype=jnp.float32), axis=1) > 0
    expert_mask = jnp.repeat(group_mask, N_EXPERTS // N_GROUPS, axis=1)
    _, idx = lax.top_k(jnp.where(expert_mask, sel, -jnp.inf), TOP_K)
    w = jnp.take_along_axis(scores, idx, axis=1)
    w = ROUTED_SCALE * w / jnp.sum(w, axis=-1, keepdims=True)
    gates = jnp.sum(jax.nn.one_hot(idx, N_EXPERTS, dtype=jnp.float32) * w[..., None], axis=1)

    def expert_step(acc, xs):
        wg, wu, wd, g = xs
        return acc + g[:, None].astype(t.dtype) * swiglu(t, wg, wu, wd), None

    routed, _ = lax.scan(expert_step, jnp.zeros_like(t), (p['w_e_gate'], p['w_e_up'], p['w_e_down'], gates.T))
    shared = swiglu(t, p['w_s_gate'], p['w_s_up'], p['w_s_down'])
    return (routed + shared).reshape(bsz, length, dim)


def setup_inputs(seed: int = 0) -> dict:
    key = jax.random.key(seed)
    ks = jax.random.split(key, 40)
    f32 = jnp.float32

    def nrm(k, shape, scale):
        return scale * jax.random.normal(k, shape, f32)

    def gain(k, shape):
        return 1.0 + 0.1 * jax.random.normal(k, shape, f32)

    lam_u = jax.random.uniform(ks[22], (DEPTH, 2, D_RNN), f32, 0.9, 0.999)
    lam_s = lam_u ** (1.0 / LRU_C)
    lru_lambda = jnp.log(lam_s) - jnp.log1p(-lam_s)
    return {
        'x': nrm(ks[0], (BATCH, SEQ, D_MODEL), 1.0),
        'c': nrm(ks[1], (BATCH, D_MODEL), 1.0),
        'ctx': nrm(ks[2], (BATCH, CTX_LEN, D_MODEL), 1.0),
        'c_ctx': nrm(ks[3], (D_MODEL,), 1.0),
        'w_mod': nrm(ks[4], (DEPTH, D_MODEL, N_MOD), 0.5 * D_MODEL ** -0.5),
        'b_mod': nrm(ks[5], (DEPTH, N_MOD), 0.02),
        'pre1_g': gain(ks[6], (DEPTH, D_MODEL)),
        'post1_g': gain(ks[7], (DEPTH, D_MODEL)),
        'pre2_g': gain(ks[8], (DEPTH, D_MODEL)),
        'post2_g': gain(ks[9], (DEPTH, D_MODEL)),
        'w_in': nrm(ks[10], (DEPTH, D_MODEL, N_IN), D_MODEL ** -0.5),
        'w_dw': nrm(ks[11], (DEPTH, CONV_WIDTH, D_CONV), CONV_WIDTH ** -0.5),
        'b_dw': nrm(ks[12], (DEPTH, D_CONV), 0.02),
        'ln_conv_g': gain(ks[13], (DEPTH, D_CONV)),
        'ln_conv_b': nrm(ks[14], (DEPTH, D_CONV), 0.02),
        'w_conv_out': nrm(ks[15], (DEPTH, D_CONV, D_MODEL), D_CONV ** -0.5),
        'w_sc': nrm(ks[16], (DEPTH, 2, SHORT_CONV, D_RNN), SHORT_CONV ** -0.5),
        'b_sc': nrm(ks[17], (DEPTH, 2, D_RNN), 0.02),
        'w_rg_a': nrm(ks[18], (DEPTH, 2, RNN_HEADS, RNN_BLOCK, RNN_BLOCK), RNN_BLOCK ** -0.5),
        'b_rg_a': nrm(ks[19], (DEPTH, 2, D_RNN), 0.02),
        'w_rg_x': nrm(ks[20], (DEPTH, 2, RNN_HEADS, RNN_BLOCK, RNN_BLOCK), RNN_BLOCK ** -0.5),
        'b_rg_x': nrm(ks[21], (DEPTH, 2, D_RNN), 0.02),
        'lru_lambda': lru_lambda,
        'w_rnn_out': nrm(ks[23], (DEPTH, D_RNN, D_MODEL), D_RNN ** -0.5),
        'w_out': nrm(ks[24], (DEPTH, D_MODEL, D_MODEL), D_MODEL ** -0.5),
        'w_router': nrm(ks[25], (DEPTH, D_MODEL, N_EXPERTS), D_MODEL ** -0.5),
        'router_bias': nrm(ks[26], (DEPTH, N_EXPERTS), 0.01),
        'w_e_gate': nrm(ks[27], (DEPTH, N_EXPERTS, D_MODEL, D_EXPERT), D_MODEL ** -0.5),
        'w_e_up': nrm(ks[28], (DEPTH, N_EXPERTS, D_MODEL, D_EXPERT), D_MODEL ** -0.5),
        'w_e_down': nrm(ks[29], (DEPTH, N_EXPERTS, D_EXPERT, D_MODEL), D_EXPERT ** -0.5),
        'w_s_gate': nrm(ks[30], (DEPTH, D_MODEL, D_SHARED), D_MODEL ** -0.5),
        'w_s_up': nrm(ks[31], (DEPTH, D_MODEL, D_SHARED), D_MODEL ** -0.5),
        'w_s_down': nrm(ks[32], (DEPTH, D_SHARED, D_MODEL), D_SHARED ** -0.5),
    }


def reference(x, c, ctx, c_ctx, w_mod, b_mod, pre1_g, post1_g, pre2_g, post2_g, w_in, w_dw, b_dw, ln_conv_g, ln_conv_b, w_conv_out, w_sc, b_sc, w_rg_a, b_rg_a, w_rg_x, b_rg_x, lru_lambda, w_rnn_out, w_out, w_router, router_bias, w_e_gate, w_e_up, w_e_down, w_s_gate, w_s_up, w_s_down):
    rows = x.shape[1] // GRID_W
    zero_state = jnp.zeros((x.shape[0], D_RNN), jnp.float32)
    cx = ctx
    for l in range(DEPTH):
        p = {
            'w_in': w_in[l], 'w_dw': w_dw[l], 'b_dw': b_dw[l], 'ln_conv_g': ln_conv_g[l], 'ln_conv_b': ln_conv_b[l],
            'w_conv_out': w_conv_out[l], 'w_sc': w_sc[l], 'b_sc': b_sc[l], 'w_rg_a': w_rg_a[l], 'b_rg_a': b_rg_a[l],
            'w_rg_x': w_rg_x[l], 'b_rg_x': b_rg_x[l], 'lru_lambda': lru_lambda[l], 'w_rnn_out': w_rnn_out[l],
            'w_out': w_out[l], 'w_router': w_router[l], 'router_bias': router_bias[l], 'w_e_gate': w_e_gate[l],
            'w_e_up': w_e_up[l], 'w_e_down': w_e_down[l], 'w_s_gate': w_s_gate[l], 'w_s_up': w_s_up[l],
            'w_s_down': w_s_down[l],
        }
        last = l == DEPTH - 1
        mod_x = jax.nn.silu(c) @ w_mod[l] + b_mod[l]
        mod_c = (jax.nn.silu(c_ctx) @ w_mod[l] + b_mod[l])[None, :]
        sh1x, sc1x, g1x, sh2x, sc2x, g2x = jnp.split(mod_x, 6, axis=-1)
        sh1c, sc1c, g1c, sh2c, sc2c, g2c = jnp.split(mod_c, 6, axis=-1)

        hc = modulate(rms_norm(cx, pre1_g[l]), sh1c, sc1c)
        if last:
            hcf, hcb = rglru_bidir(hc @ w_in[l][:, OFF_RNN_X:OFF_RNN_G], p, zero_state, zero_state)
        else:
            out_c, hcf, hcb = mixer(hc, p, None, zero_state, zero_state)
            cx = cx + g1c[:, None, :] * rms_norm(out_c, post1_g[l])
            hc2 = modulate(rms_norm(cx, pre2_g[l]), sh2c, sc2c)
            cx = cx + g2c[:, None, :] * rms_norm(moe(hc2, p), post2_g[l])
        h0_f = hcf[:, -1]
        h0_b = hcb[:, 0]

        hx = modulate(rms_norm(x, pre1_g[l]), sh1x, sc1x)
        out_x, _, _ = mixer(hx, p, rows, h0_f, h0_b)
        x = x + g1x[:, None, :] * rms_norm(out_x, post1_g[l])
        hx2 = modulate(rms_norm(x, pre2_g[l]), sh2x, sc2x)
        x = x + g2x[:, None, :] * rms_norm(moe(hx2, p), post2_g[l])
    return x
```

```python
import numpy as np
from contextlib import ExitStack
import concourse.bass as bass
import concourse.mybir as mybir
from concourse.bass_utils import run_bass_kernel_spmd

F32 = mybir.dt.float32
BF16 = mybir.dt.bfloat16
AF = mybir.ActivationFunctionType
ALU = mybir.AluOpType
AX = mybir.AxisListType
EPS = 1e-6

VROWS = (['pre1_g', 'pre2_g', 'b_dw', 'ln_g', 'ln_b'] + ['dw%d' % i for i in range(31)]
         + ['scP%d' % i for i in range(4)] + ['scQ%d' % i for i in range(4)]
         + ['bscP', 'bscQ', 'braP', 'braQ', 'brxP', 'brxQ', 'lamP', 'lamQ',
            'bm_sh1', 'bm_sc1', 'bm_sh2', 'bm_sc2'])
VIDX = {n: i for i, n in enumerate(VROWS)}
NV = len(VROWS) * 8
NEXP = 65


class Prog:
    def __init__(self, nc, es):
        self.nc = nc
        self.es = es
        self.engs = ["tensor", "vector", "scalar", "gpsimd", "sync"]
        self.sem = {}
        for e in self.engs:
            self.sem["E_" + e] = es.enter_context(nc.semaphore("E_" + e))
        self.cnt = {e: 0 for e in self.engs}
        self.dcnt = {}
        self.ops = {e: [] for e in self.engs}
        self.waited = {e: {} for e in self.engs}
        self.lastw = {}
        self.rd = {}
        self.pend = {e: [] for e in self.engs}

    def _deps(self, eng, reads, writes):
        deps = {}

        def add(st):
            if st is None:
                return
            s, v = st
            if eng == "tensor" and s == "E_tensor":
                return
            if deps.get(s, 0) < v:
                deps[s] = v
        for r in reads:
            add(self.lastw.get(r))
        for w in writes:
            add(self.lastw.get(w))
            for s, v in self.rd.get(w, {}).items():
                add((s, v))
        waits = []
        wd = self.waited[eng]
        for s, v in deps.items():
            if wd.get(s, 0) < v:
                wd[s] = v
                waits.append((s, v))
        return waits

    def _stamp(self, st, reads, writes):
        s, v = st
        for r in reads:
            d = self.rd.setdefault(r, {})
            if d.get(s, 0) < v:
                d[s] = v
        for w in writes:
            self.lastw[w] = st
            self.rd[w] = {}

    def op(self, eng, fn, reads=(), writes=(), inc=True, dma=None):
        reads = tuple(reads)
        writes = tuple(writes)
        for e2 in self.engs:
            if e2 != eng and self.pend[e2]:
                for (pr, pw) in self.pend[e2]:
                    for k in reads + writes:
                        assert k not in pw, ("pending write hazard", k)
                    for k in writes:
                        assert k not in pr, ("pending read hazard", k)
        waits = self._deps(eng, reads, writes)
        if dma is not None:
            if dma not in self.sem:
                self.sem[dma] = self.es.enter_context(self.nc.semaphore(dma))
                self.dcnt[dma] = 0
            self.dcnt[dma] += 16
            st = (dma, self.dcnt[dma])
            self.ops[eng].append((waits, fn, ("dma", dma)))
            self._stamp(st, reads, writes)
        elif inc:
            self.cnt[eng] += 1
            st = ("E_" + eng, self.cnt[eng])
            self.ops[eng].append((waits, fn, ("inc", "E_" + eng)))
            for (r, w) in self.pend[eng]:
                self._stamp(st, r, w)
            self.pend[eng] = []
            self._stamp(st, reads, writes)
        else:
            self.ops[eng].append((waits, fn, None))
            self.pend[eng].append((reads, writes))

    def run(self):
        nc = self.nc
        for e in self.engs:
            assert not self.pend[e], e
        with nc.Block() as block:
            for e in self.engs:
                ops = self.ops[e]

                def body(eng, ops=ops):
                    for waits, fn, post in ops:
                        for s, v in waits:
                            eng.wait_ge(self.sem[s], v)
                        ins = fn(eng)
                        if post is not None:
                            kind, s = post
                            ins.then_inc(self.sem[s], 16 if kind == "dma" else 1)
                getattr(block, e)(body)
        self.ops = {e: [] for e in self.engs}

    def mm(self, out, lhsT, rhs, start, stop, R, W, inc):
        self.op("tensor", lambda e: e.matmul(out, lhsT=lhsT, rhs=rhs, start=start, stop=stop), R, W, inc=inc)

    def tp(self, out, in_, ident, R, W, inc):
        self.op("tensor", lambda e: e.transpose(out, in_, ident), R, W, inc=inc)

    def act(self, out, in_, func, R, W, bias=None, scale=None, accum=None):
        kw = {}
        if bias is not None:
            kw["bias"] = bias
        if scale is not None:
            kw["scale"] = scale
        if accum is not None:
            kw["accum_out"] = accum
        self.op("scalar", lambda e: e.activation(out=out, in_=in_, func=func, **kw), R, W)

    def tt(self, eng, out, in0, in1, op, R, W):
        self.op(eng, lambda e: e.tensor_tensor(out=out, in0=in0, in1=in1, op=op), R, W)

    def ts(self, eng, out, in0, s1, s2, op0, op1, R, W):
        if op1 is None:
            self.op(eng, lambda e: e.tensor_scalar(out=out, in0=in0, scalar1=s1, scalar2=None, op0=op0), R, W)
        else:
            self.op(eng, lambda e: e.tensor_scalar(out=out, in0=in0, scalar1=s1, scalar2=s2, op0=op0, op1=op1), R, W)

    def stt(self, out, in0, scalar, in1, op0, op1, R, W):
        self.op("vector", lambda e: e.scalar_tensor_tensor(out=out, in0=in0, scalar=scalar, in1=in1, op0=op0, op1=op1), R, W)

    def copy(self, eng, out, in_, R, W):
        self.op(eng, lambda e: e.tensor_copy(out=out, in_=in_), R, W)

    def memset(self, eng, ap, val, W):
        self.op(eng, lambda e: e.memset(ap, val), (), W)

    def dma(self, eng, out, in_, R, W, sem, **kw):
        self.op(eng, lambda e: e.dma_start(out=out, in_=in_, **kw), R, W, dma=sem)


def build(dbg=False, stop=None):
    nc = bass.Bass("TRN2", target_bir_lowering=False)

    def din(name, shape):
        return nc.dram_tensor(name, shape, F32, kind="ExternalInput").ap()
    xs = din("xs", [4096, 1024])
    ctxs = din("ctxs", [256, 1024])
    cvT = din("cvT", [128, 16])
    vecs = din("vecs", [128, NV])
    rows = din("rows", [4, 1024])
    rbias = din("rbias", [1, 64])
    identd = din("ident", [128, 128])
    w_mod = din("w_mod", [1024, 6144])
    w_in = din("w_in", [1024, 6144])
    w_rg = din("w_rg", [16, 256, 256])
    w_co = din("w_co", [1024, 1024])
    w_ro = din("w_ro", [1024, 1024])
    w_o = din("w_o", [1024, 1024])
    w_rt = din("w_rt", [1024, 64])
    wg = din("wg", [NEXP, 1024, 256])
    wu = din("wu", [NEXP, 1024, 256])
    wd = din("wd", [NEXP, 256, 1024])
    out = nc.dram_tensor("out", [2048, 1024], F32, kind="ExternalOutput").ap()
    sk_ = "ExternalOutput" if dbg else "Internal"
    hs_scr = nc.dram_tensor("hs_scr", [8, 128, 2048], F32, kind=sk_).ap()
    gscr = nc.dram_tensor("gscr", [NEXP, 2048], F32, kind=sk_).ap()
    dbgo = nc.dram_tensor("dbgo", [128, 4096], F32, kind="ExternalOutput").ap() if dbg else None

    with ExitStack() as es:
        P = Prog(nc, es)

        def sb(stack, name, shape, dt):
            return stack.enter_context(nc.sbuf_tensor(name, shape, dt))

        def ps(stack, name, shape, dt):
            return stack.enter_context(nc.psum_tensor(name, shape, dt))

        vec = sb(es, "vec", [128, NV], F32)
        cv = sb(es, "cv", [128, 16], F32)
        identf = sb(es, "identf", [128, 128], F32)
        identb = sb(es, "identb", [128, 128], BF16)
        onesf = sb(es, "onesf", [128, 128], F32)
        gvec1 = sb(es, "gvec1", [128, 1024], F32)
        gvec2 = sb(es, "gvec2", [128, 1024], F32)
        rbtmp = sb(es, "rbtmp", [128, 1024], F32)
        rbias_sb = sb(es, "rbias_sb", [128, 64], F32)
        mc = sb(es, "mc", [128, 6, 8], F32)
        clc = sb(es, "clc", [128, 2, 16], F32)
        hxT = sb(es, "hxT", [128, 8, 2048], BF16)
        wrt16 = sb(es, "wrt16", [128, 8, 64], BF16)
        small = sb(es, "small", [128, 64], F32)
        xn16s = [sb(es, "xn16_%d" % i, [128, 1024], BF16) for i in range(2)]
        xni = [0]
        junk16 = sb(es, "junk16", [128, 1024], BF16)
        modtmp = sb(es, "modtmp", [128, 8, 128], F32)
        xt = [sb(es, "xt%d" % i, [128, 1024], F32) for i in range(2)]

        def vcol(name, c=None):
            b = VIDX[name] * 8
            if c is None:
                return vec[:, b:b + 8]
            return vec[:, b + c:b + c + 1]

        cst_keys = ["vec", "cv", "identf", "gvec1", "rbtmp", "rbias"]
        P.dma("sync", vec[:], vecs, (), ["vec"], "cst")
        P.dma("sync", cv[:], cvT, (), ["cv"], "cst")
        P.dma("sync", identf[:], identd, (), ["identf"], "cst")
        P.dma("sync", gvec1[:].unsqueeze(1), rows[1:2, :].partition_broadcast(128), (), ["gvec1"], "cst")
        P.dma("sync", rbtmp[:].unsqueeze(1), rows[0:1, :].partition_broadcast(128), (), ["rbtmp"], "cst")
        P.dma("sync", rbias_sb[:].unsqueeze(1), rbias[0:1, :].partition_broadcast(128), (), ["rbias"], "cst")
        for k in cst_keys:
            P.lastw[k] = ("cst", P.dcnt["cst"])
        P.copy("vector", identb[:], identf[:], ["identf"], ["identb"])
        P.memset("vector", onesf[:], 1.0, ["onesf"])

        xt_i = [0]

        def norm_pre(src_dram, src_sb, src_key):
            if src_sb is None:
                b = xt_i[0] % 2
                xt_i[0] += 1
                src_sb = xt[b]
                src_key = "xt%d" % b
                P.dma("sync", src_sb[:], src_dram, (), [src_key], src_key)
            xb_ = xni[0] % 2
            xni[0] += 1
            xn16, xnk = xn16s[xb_], "xn16_%d" % xb_
            P.act(junk16[:], src_sb[:], AF.Square, [src_key], ["junk16", "ss"], accum=small[:, 0:1])
            P.act(small[:, 1:2], small[:, 0:1], AF.Ln, ["ss"], ["sq"], scale=1.0 / 1024, bias=EPS)
            P.act(small[:, 2:3], small[:, 1:2], AF.Exp, ["sq"], ["rstd"], scale=-0.5)
            P.act(xn16[:], src_sb[:], AF.Copy, [src_key, "rstd"], [xnk], scale=small[:, 2:3])
            return xn16, xnk

        def norm_post(xn, Aidx, dst_col, dst_key, tbank, tbkey):
            xn16, xnk = xn
            for c in range(8):
                P.tp(tbank[:, c * 128:(c + 1) * 128], xn16[:, c * 128:(c + 1) * 128], identb[:],
                     [xnk, "identb"], [tbkey], inc=(c == 7))
            tv = tbank[:].rearrange("p (c t) -> p c t", t=128)
            Ab = mc[:, Aidx, :].unsqueeze(2).to_broadcast([128, 8, 128])
            Bb = mc[:, Aidx + 1, :].unsqueeze(2).to_broadcast([128, 8, 128])
            P.tt("vector", modtmp[:], tv, Ab, ALU.mult, [tbkey, "mc"], ["modtmp"])
            dst, dkey = dst_col
            P.tt("vector", dst, modtmp[:], Bb, ALU.add, ["modtmp", "mc"], dst_key if isinstance(dst_key, list) else [dst_key])

        def norm_tile(src_dram, src_sb, src_key, Aidx, dst_col, dst_key, tbank, tbkey):
            xn = norm_pre(src_dram, src_sb, src_key)
            norm_post(xn, Aidx, dst_col, dst_key, tbank, tbkey)

        with ExitStack() as ea:
            S0 = sb(ea, "S0", [128, 8192], BF16)
            S1 = sb(ea, "S1", [128, 8192], BF16)
            dsc = sb(ea, "dsc", [128, 64, 128], BF16)
            sc32 = sb(ea, "sc32", [128, 16], F32)
            sc16 = sb(ea, "sc16", [128, 16], BF16)
            screp = sb(ea, "screp", [128, 8, 128], BF16)
            modfm = sb(ea, "modfm", [128, 64], F32)
            ltmp = sb(ea, "ltmp", [128, 3, 16], F32)
            hb = sb(ea, "hb", [128, 32], F32)
            hTt = sb(ea, "hTt", [128, 8, 512], BF16)
            u16s = [sb(ea, "u16_%d" % i, [128, 8, 518], BF16) for i in range(2)]
            svP = sb(ea, "svP", [128, 8, 3], BF16)
            svQ = sb(ea, "svQ", [128, 8, 3], BF16)
            v32 = sb(ea, "v32", [128, 8, 512], F32)
            v16 = sb(ea, "v16", [128, 8, 512], BF16)
            hout = sb(ea, "hout", [128, 8, 512], F32)
            gt = [[sb(ea, "g%s%d" % (n, i), [128, 512], F32) for n in "riam"] for i in range(3)]
            stt_ = sb(ea, "state", [128, 2, 8], F32)
            pb = [ps(ea, "pb%d" % i, [128, 512], F32) for i in range(6)]
            tb = [ps(ea, "tb%d" % i, [128, 1024], BF16) for i in range(2)]
            pbi = [0]

            def nextpb():
                i = pbi[0] % 6
                pbi[0] += 1
                return pb[i], "pb%d" % i
            tbi = [0]

            def nexttb():
                i = tbi[0] % 2
                tbi[0] += 1
                return tb[i], "tb%d" % i

            S = [S0, S1]

            def slotv(i):
                return S[i][:].rearrange("p (k n) -> p k n", n=1024)

            P.act(sc32[:], cv[:], AF.Silu, ["cv"], ["sc32"])
            P.copy("vector", sc16[:], sc32[:], ["sc32"], ["sc16"])
            P.copy("vector", screp[:], sc32[:, 0:8].unsqueeze(2).to_broadcast([128, 8, 128]), ["sc32"], ["screp"])
            modps, modk = nextpb()
            fm_i = 0
            for q in range(6):
                si = q % 2
                sv = slotv(si)
                P.dma("gpsimd", sv, w_mod[:, q * 1024:(q + 1) * 1024].rearrange("(k p) n -> p k n", p=128),
                      (), ["S%d" % si], "S%d" % si)
                if q in (0, 1, 3, 4):
                    for j in range(8):
                        col = (fm_i * 8 + j) * 2
                        for kc in range(8):
                            P.mm(modps[:, col:col + 2], sv[:, kc, j * 128:(j + 1) * 128], sc16[:, kc:16:8],
                                 kc == 0, kc == 7, ["S%d" % si, "sc16"], [modk], inc=(j == 7 and kc == 7))
                    fm_i += 1
                else:
                    gv = gvec1 if q == 2 else gvec2
                    gk = "gvec1" if q == 2 else "gvec2"
                    if q == 5:
                        P.dma("sync", gvec2[:].unsqueeze(1), rows[3:4, :].partition_broadcast(128), (), ["gvec2"], "cst2")
                        P.dma("sync", rbtmp[:].unsqueeze(1), rows[2:3, :].partition_broadcast(128), (), ["rbtmp"], "cst3")
                    for half in range(2):
                        gp, gpk = nextpb()
                        for kc in range(8):
                            P.mm(gp[:], screp[:, kc, :], sv[:, kc, half * 512:(half + 1) * 512], kc == 0, kc == 7,
                                 ["S%d" % si, "screp"], [gpk], inc=(kc == 7))
                        hs_ = slice(half * 512, (half + 1) * 512)
                        P.tt("vector", gv[:, hs_], gp[:], gv[:, hs_], ALU.add, [gpk, gk], [gk])
                        P.tt("vector", gv[:, hs_], gv[:, hs_], rbtmp[:, hs_], ALU.mult, [gk, "rbtmp"], [gk])
            P.act(modfm[:], modps[:, 0:64], AF.Copy, [modk], ["modfm"])
            mv = modfm[:].rearrange("p (q j t) -> p q j t", q=4, j=8)
            P.tt("vector", mc[:, 1, :], mv[:, 0, :, 0], vcol("bm_sh1"), ALU.add, ["modfm", "vec"], ["mc"])
            P.tt("vector", mc[:, 3, :], mv[:, 0, :, 1], vcol("bm_sh1"), ALU.add, ["modfm", "vec"], ["mc"])
            P.tt("vector", mc[:, 5, :], mv[:, 2, :, 0], vcol("bm_sh2"), ALU.add, ["modfm", "vec"], ["mc"])
            for (dst, qi, t, bmn, gn) in ((0, 1, 0, "bm_sc1", "pre1_g"), (2, 1, 1, "bm_sc1", "pre1_g"), (4, 3, 0, "bm_sc2", "pre2_g")):
                P.tt("vector", mc[:, dst, :], mv[:, qi, :, t], vcol(bmn), ALU.add, ["modfm", "vec", "mc"], ["mc"])
                P.stt(mc[:, dst, :], mc[:, dst, :], 1.0, vcol(gn), ALU.add, ALU.mult, ["mc", "vec"], ["mc"])
            lb = VIDX["lamP"] * 8
            P.act(ltmp[:, 0, :], vec[:, lb:lb + 16], AF.Exp, ["vec"], ["ltmp"], scale=-1.0)
            P.ts("vector", ltmp[:, 1, :], ltmp[:, 0, :], -1.0 / 3, 0.5, ALU.mult, ALU.add, ["ltmp"], ["ltmp"])
            P.tt("vector", ltmp[:, 1, :], ltmp[:, 1, :], ltmp[:, 0, :], ALU.mult, ["ltmp"], ["ltmp"])
            P.ts("vector", ltmp[:, 1, :], ltmp[:, 1, :], -1.0, 1.0, ALU.mult, ALU.add, ["ltmp"], ["ltmp"])
            P.tt("vector", ltmp[:, 2, :], ltmp[:, 1, :], ltmp[:, 0, :], ALU.mult, ["ltmp"], ["ltmp"])
            P.ts("vector", clc[:, 0, :], ltmp[:, 2, :], -8.0, None, ALU.mult, None, ["ltmp"], ["clc"])
            P.ts("vector", clc[:, 1, :], ltmp[:, 2, :], -16.0, None, ALU.mult, None, ["ltmp"], ["clc"])
            bb0 = VIDX["braP"] * 8
            P.ts("vector", hb[:], vec[:, bb0:bb0 + 32], -1.0, None, ALU.mult, None, ["vec"], ["hb"])
            sb0 = VIDX["scP0"] * 8
            P.tt("vector", dsc[:], identb[:].unsqueeze(1).to_broadcast([128, 64, 128]),
                 vec[:, sb0:sb0 + 64].unsqueeze(2).to_broadcast([128, 64, 128]), ALU.mult, ["identb", "vec"],
                 [("dsc", i) for i in range(64)])
            wxv = slotv(0)
            P.dma("gpsimd", wxv, w_in[:, 2048:3072].rearrange("(k p) n -> p k n", p=128), (), ["S0"], "S0")
            wrgv = S1[:].rearrange("p (m n) -> p m n", n=256)
            P.dma("gpsimd", wrgv, w_rg.rearrange("m (kk p) n -> p (m kk) n", p=128), (), ["S1"], "S1")
            P.dma("gpsimd", wrt16[:], w_rt.rearrange("(k p) n -> p k n", p=128), (), ["wrt16"], "wrt")
            P.memset("vector", stt_[:], 0.0, ["stP", "stQ"])

            gset = [0]

            def stage1(src_rows, ntile, Aidx, hT, hT_c0, hT_key, do_norm, haloL, haloR, ub):
                T = ntile * 128
                u16 = u16s[ub]
                if do_norm:
                    for t in range(ntile):
                        tbk, tbkey = nexttb()
                        norm_tile(src_rows[t * 128:(t + 1) * 128, :], None, None, Aidx,
                                  (hT[:, :, hT_c0 + t * 128: hT_c0 + (t + 1) * 128], None), hT_key, tbk, tbkey)
                        yield
                for m in range(8):
                    up, upk = nextpb()
                    for kc in range(8):
                        P.mm(up[:, 0:T], wxv[:, kc, m * 128:(m + 1) * 128], hT[:, kc, hT_c0:hT_c0 + T], kc == 0, kc == 7,
                             ["S0", hT_key], [upk], inc=(kc == 7))
                    P.copy("vector", u16[:, m, 3:3 + T], up[:, 0:T], [upk], [("u16", ub, m)])
                    yield
                uk = [("u16", ub, m) for m in range(8)]
                if haloL == "zero":
                    P.memset("vector", u16[:, :, 0:3], 0.0, [("u16L", ub)])
                else:
                    P.copy("vector", u16[:, :, 0:3], svP[:], ["svP"], [("u16L", ub)])
                if haloR == "zero":
                    P.memset("vector", u16[:, :, 3 + T:6 + T], 0.0, [("u16R", ub)])
                else:
                    P.copy("vector", u16[:, :, 3 + T:6 + T], svQ[:], ["svQ"], [("u16R", ub)])
                P.copy("vector", svP[:], u16[:, :, T:T + 3], uk, ["svP"])
                P.copy("vector", svQ[:], u16[:, :, 3:6], uk, ["svQ"])
                yield

            def stage2(ntile, dirs, store, blk, ub, tick):
                T = ntile * 128
                u16 = u16s[ub]
                for d in dirs:
                    di = 0 if d == "P" else 1
                    hk = ("u16L", ub) if d == "P" else ("u16R", ub)
                    for c in range(8):
                        cp, cpk = nextpb()
                        for j in range(4):
                            off = j if d == "P" else 3 + j
                            P.mm(cp[:, 0:T], dsc[:, (di * 4 + j) * 8 + c, :], u16[:, c, off:off + T], j == 0, j == 3,
                                 [("dsc", (di * 4 + j) * 8 + c), ("u16", ub, c), hk], [cpk], inc=(j == 3))
                        P.ts("vector", v32[:, c, 0:T], cp[:, 0:T], vcol("bsc" + d, c), None, ALU.add, None, [cpk, "vec"], [("v32", c)])
                        P.copy("vector", v16[:, c, 0:T], v32[:, c, 0:T], [("v32", c)], [("v16", c)])
                        tick()
                    def ph1(m, d=d, di=di):
                        h = m // 2
                        gs = gset[0] % 3
                        gset[0] += 1
                        gr, gi, ga, gm = gt[gs]
                        kr, ki, ka, km = [("g", gs, n) for n in "riam"]
                        rp, rpk = nextpb()
                        ip, ipk = nextpb()
                        for (pp, ppk, mat) in ((rp, rpk, di * 2), (ip, ipk, di * 2 + 1)):
                            for kk in range(2):
                                P.mm(pp[:, 0:T], wrgv[:, (mat * 4 + h) * 2 + kk, (m % 2) * 128:(m % 2 + 1) * 128],
                                     v16[:, 2 * h + kk, 0:T], kk == 0, kk == 1, ["S1", ("v16", 2 * h + kk)], [ppk], inc=(kk == 1))
                        P.act(gr[:, 0:T], rp[:, 0:T], AF.Exp, [rpk, "hb"], [kr], scale=-1.0, bias=hb[:, di * 8 + m:di * 8 + m + 1])
                        P.act(gi[:, 0:T], ip[:, 0:T], AF.Exp, [ipk, "hb"], [ki], scale=-1.0, bias=hb[:, 16 + di * 8 + m:16 + di * 8 + m + 1])
                        P.act(gr[:, 0:T], gr[:, 0:T], AF.Ln, [kr], [kr], scale=1.0, bias=1.0)
                        P.act(gi[:, 0:T], gi[:, 0:T], AF.Ln, [ki], [ki], scale=1.0, bias=1.0)
                        P.act(gr[:, 0:T], gr[:, 0:T], AF.Exp, [kr], [kr], scale=-1.0)
                        c0_ = clc[:, 0, di * 8 + m:di * 8 + m + 1]
                        P.act(ga[:, 0:T], gr[:, 0:T], AF.Exp, [kr, "clc"], [ka], scale=c0_)
                        P.tt("vector", gm[:, 0:T], ga[:, 0:T], ga[:, 0:T], ALU.mult, [ka], [km])
                        return gs

                    def ph2(m, gs, d=d, di=di):
                        gr, gi, ga, gm = gt[gs]
                        kr, ki, ka, km = [("g", gs, n) for n in "riam"]
                        P.act(gm[:, 0:T], gm[:, 0:T], AF.Ln, [km], [km], scale=-1.0, bias=1.0)
                        P.stt(gi[:, 0:T], gm[:, 0:T], 0.5, gi[:, 0:T], ALU.mult, ALU.subtract, [km, ki], [ki])

                    def ph3(m, gs, d=d, di=di):
                        gr, gi, ga, gm = gt[gs]
                        kr, ki, ka, km = [("g", gs, n) for n in "riam"]
                        gb, kb = gi, ki
                        P.act(gi[:, 0:T], gi[:, 0:T], AF.Exp, [ki], [ki])
                        P.tt("vector", gi[:, 0:T], gi[:, 0:T], v32[:, m, 0:T], ALU.mult, [ki, ("v32", m)], [ki])
                        stk = "st" + d
                        if d == "P":
                            o_, a_, b_ = hout[:, m, 0:T], ga[:, 0:T], gb[:, 0:T]
                            last = hout[:, m, T - 1:T]
                        else:
                            o_, a_, b_ = hout[:, m, 0:T][:, ::-1], ga[:, 0:T][:, ::-1], gb[:, 0:T][:, ::-1]
                            last = hout[:, m, 0:1]
                        init = stt_[:, di, m:m + 1]
                        P.op("vector", lambda e, o_=o_, a_=a_, b_=b_, init=init: e.tensor_tensor_scan(
                            out=o_, data0=a_, data1=b_, initial=init, op0=ALU.mult, op1=ALU.add),
                            [ka, kb, (stk, m)], [("hout", m)])
                        P.copy("vector", stt_[:, di, m:m + 1], last, [("hout", m)], [(stk, m)])
                        if store == "write":
                            P.dma("sync", hs_scr[m, :, blk * 512:(blk + 1) * 512], hout[:, m, :], [("hout", m)],
                                  [("hs", m, blk)], "hs%d" % m)
                        elif store == "accum":
                            P.dma("gpsimd", hs_scr[m, :, blk * 512:(blk + 1) * 512], hout[:, m, :], [("hout", m)],
                                  [("hs", m, blk)], "hs%d" % m, accum_op=ALU.add)
                        tick()

                    q1 = None
                    q2 = None
                    for m in range(8 + 2):
                        new1 = (m, ph1(m)) if m < 8 else None
                        if q1 is not None:
                            ph2(*q1)
                        if q2 is not None:
                            ph3(*q2)
                        q2 = q1
                        q1 = new1

            for m in range(8):
                P.lastw[("stP", m)] = P.lastw["stP"]
                P.lastw[("stQ", m)] = P.lastw["stQ"]
            passes = []
            passes.append(dict(src=ctxs, ntile=2, Aidx=2, hT=hTt, c0=0, hk="hTt", norm=True, dirs=["P", "Q"], hL="zero", hR="zero", store=None, blk=0))
            for j in range(4):
                passes.append(dict(src=xs[j * 512:(j + 1) * 512, :], ntile=4, Aidx=0, hT=hxT, c0=j * 512, hk=("hxT", j), norm=True, dirs=["P"],
                                   hL="zero" if j == 0 else "chain", hR="zero", store="write", blk=j))
            for j in (7, 6, 5, 4):
                passes.append(dict(src=xs[j * 512:(j + 1) * 512, :], ntile=4, Aidx=0, hT=hTt, c0=0, hk="hTt", norm=True, dirs=["Q"],
                                   hL="zero", hR="zero" if j == 7 else "chain", store=None, blk=j))
            for j in (3, 2, 1, 0):
                passes.append(dict(src=None, ntile=4, Aidx=0, hT=hxT, c0=j * 512, hk=("hxT", j), norm=False, dirs=["Q"],
                                   hL="zero", hR="chain", store="accum", blk=j))

            def mk1(k):
                p = passes[k]
                return stage1(p["src"], p["ntile"], p["Aidx"], p["hT"], p["c0"], p["hk"], p["norm"], p["hL"], p["hR"], k % 2)
            for _ in mk1(0):
                pass
            for k, p in enumerate(passes):
                gen = mk1(k + 1) if k + 1 < len(passes) else iter(())

                def tick(gen=gen):
                    next(gen, None)
                stage2(p["ntile"], p["dirs"], p["store"], p["blk"], k % 2, tick)
                for _ in gen:
                    pass
            if dbg:
                P.copy("vector", modtmp[:, 0:6, 0:8], mc[:], ["mc"], ["dbgt"])
                P.copy("vector", modtmp[:, 6:8, 0:8], stt_[:], [("stP", m) for m in range(8)] + [("stQ", m) for m in range(8)], ["dbgt"])
                P.dma("sync", dbgo[:, 0:1024], modtmp[:].rearrange("p a b -> p (a b)"), ["dbgt"], ["dbgo"], "dbg")
                P.dma("sync", dbgo[:, 1024:2048], gvec1[:], ["gvec1"], ["dbgo1"], "dbg1")
                P.dma("sync", dbgo[:, 2048:3072], gvec2[:], ["gvec2"], ["dbgo2"], "dbg2")
                P.op("sync", lambda e: None, ["dbgo", "dbgo1", "dbgo2"] + [("hs", m, j) for m in range(8) for j in range(4)], (), inc=False)
                P.pend["sync"] = []
            P.run()
        if stop == "A":
            return nc

        with ExitStack() as eb:
            NS = 3
            ring = [sb(eb, "R%d" % i, [128, 8, 1024], BF16) for i in range(NS)]
            ucp = sb(eb, "ucp", [128, 8, 8, 94], BF16)
            big32 = sb(eb, "big32", [128, 8, 512], F32)
            za = sb(eb, "za", [128, 8, 512], BF16)
            zb = sb(eb, "zb", [128, 8, 512], BF16)
            hsb = [sb(eb, "hsb%d" % i, [128, 512], F32) for i in range(2)]
            ddw2 = [sb(eb, "ddw%d" % i, [128, 31, 128], BF16) for i in range(2)]
            ddi = [0]
            tA = [sb(eb, "tA%d" % i, [128, 512], F32) for i in range(3)]
            mean_sb = sb(eb, "mean_sb", [128, 512], F32)
            rstd_sb = sb(eb, "rstd_sb", [128, 512], F32)
            x1t = [sb(eb, "x1t%d" % i, [128, 1024], F32) for i in range(2)]
            gT = sb(eb, "gT", [128, 2048], F32)
            tk = sb(eb, "tk", [128, 8, 64], F32)
            dbgst = sb(eb, "dbgst", [128, 512], F32) if dbg else None
            dslot = [0]

            def dump(ap, keys):
                if not dbg or dslot[0] >= 8:
                    return
                i = dslot[0]
                dslot[0] += 1
                P.copy("vector", dbgst[:], ap, keys, ["dbgst"])
                P.dma("sync", dbgo[:, i * 512:(i + 1) * 512], dbgst[:], ["dbgst"], ["dbgo"], "dbgB")
            pb = [ps(eb, "qb%d" % i, [128, 512], F32) for i in range(7)]
            tbB = ps(eb, "tbB", [128, 1024], BF16)
            pbi = [0]

            def nextpb():
                i = pbi[0] % 5
                pbi[0] += 1
                return pb[i], "qb%d" % i
            tai = [0]

            def nexttA():
                i = tai[0] % 3
                tai[0] += 1
                return tA[i], "tA%d" % i

            loads = []
            for j in range(4):
                loads += [(w_in, 0), (w_in, 1024), (w_co, 0), (w_in, 4096), (w_in, 3072), (w_ro, 0), (w_in, 5120), (w_o, 0)]
            issued = [0]

            def need(k, done):
                while issued[0] <= min(done - 1 + NS, len(loads) - 1):
                    i = issued[0]
                    w_, c0 = loads[i]
                    s = i % NS
                    P.dma("gpsimd", ring[s][:], w_[:, c0:c0 + 1024].rearrange("(k p) n -> p k n", p=128),
                          (), ["R%d" % s], "R%d" % s)
                    issued[0] += 1
                assert issued[0] > k
                return ring[k % NS], "R%d" % (k % NS)

            P.memset("vector", ucp[:], 0.0, [("ucp", c) for c in range(8)])
            P.memset("vector", gT[:], 1.0, ["gT"])
            dwb = VIDX["dw0"] * 8
            li = 0
            pending_tail = [iter(())]
            for j in range(4):
                blk = slice(j * 512, (j + 1) * 512)
                hk = ("hxT", j)
                Wa, Wak = need(li, li if j == 0 else li - 1)
                Wg, Wgk = need(li + 1, li if j == 0 else li - 1)
                for c in range(8):
                    ap_, apk = nextpb()
                    gp_, gpk = nextpb()
                    for (pp, ppk, W_, Wk_) in ((ap_, apk, Wa, Wak), (gp_, gpk, Wg, Wgk)):
                        for kc in range(8):
                            P.mm(pp[:], W_[:, kc, c * 128:(c + 1) * 128], hxT[:, kc, blk], kc == 0, kc == 7,
                                 [Wk_, hk], [ppk], inc=(kc == 7))
                    t_, tk_ = nexttA()
                    P.act(t_[:], gp_[:], AF.Sigmoid, [gpk], [tk_])
                    P.tt("vector", ucp[:, c, :, 15:79], ap_[:].rearrange("p (r t) -> p r t", t=64),
                         t_[:].rearrange("p (r t) -> p r t", t=64), ALU.mult, [apk, tk_], [("ucp", c)])
                    next(pending_tail[0], None)
                    next(pending_tail[0], None)
                for _ in pending_tail[0]:
                    pass
                mp, mpk = pb[5], "qb5"
                sp_, spk = pb[6], "qb6"
                for c in range(8):
                    ddw = ddw2[ddi[0] % 2]
                    ddk = "ddw%d" % (ddi[0] % 2)
                    ddi[0] += 1
                    P.tt("vector", ddw[:], identb[:].unsqueeze(1).to_broadcast([128, 31, 128]),
                         vec[:, dwb + c:dwb + 31 * 8:8].unsqueeze(2).to_broadcast([128, 31, 128]), ALU.mult,
                         ["identb", "vec"], [ddk])
                    cp, cpk = nextpb()
                    for tap in range(31):
                        P.mm(cp[:].rearrange("p (r t) -> p r t", t=64), ddw[:, tap, :], ucp[:, c, :, tap:tap + 64], tap == 0, tap == 30,
                             [ddk, ("ucp", c)], [cpk], inc=(tap == 30))
                    P.act(big32[:, c, :], cp[:], AF.Identity, [cpk, "vec"], [("big32", c)], bias=vcol("b_dw", c))
                    if j == 0 and c == 0:
                        dump(big32[:, 0, :], [("big32", 0)])
                    t_, tk_ = nexttA()
                    P.act(t_[:], cp[:], AF.Square, [cpk, "vec"], [tk_], bias=vcol("b_dw", c))
                    P.mm(mp[:], onesf[:], big32[:, c, :], c == 0, c == 7, ["onesf", ("big32", c)], [mpk], inc=(c == 7))
                    P.mm(sp_[:], onesf[:], t_[:], c == 0, c == 7, ["onesf", tk_], [spk], inc=(c == 7))
                P.ts("vector", mean_sb[:], mp[:], 1.0 / 1024, None, ALU.mult, None, [mpk], ["mean_sb"])
                P.tt("vector", rstd_sb[:], mean_sb[:], mean_sb[:], ALU.mult, ["mean_sb"], ["rstd_sb"])
                P.stt(rstd_sb[:], sp_[:], 1.0 / 1024, rstd_sb[:], ALU.mult, ALU.subtract, [spk, "rstd_sb"], ["rstd_sb"])
                P.act(rstd_sb[:], rstd_sb[:], AF.Sqrt, ["rstd_sb"], ["rstd_sb"], bias=EPS, scale=1.0)
                P.op("vector", lambda e: e.reciprocal(out=rstd_sb[:], in_=rstd_sb[:]), ["rstd_sb"], ["rstd_sb"])
                for c in range(8):
                    P.tt("vector", big32[:, c, :], big32[:, c, :], mean_sb[:], ALU.subtract, [("big32", c), "mean_sb"], [("big32", c)])
                    P.tt("vector", big32[:, c, :], big32[:, c, :], rstd_sb[:], ALU.mult, [("big32", c), "rstd_sb"], [("big32", c)])
                    P.act(za[:, c, :], big32[:, c, :], AF.Silu, [("big32", c), "vec"], [("za", c)],
                          scale=vcol("ln_g", c), bias=vcol("ln_b", c))
                if j == 0:
                    dump(za[:, 0, :], [("za", 0)])
                    dump(za[:, 1, :], [("za", 1)])
                Wco, Wcok = need(li + 2, li + 2)
                Wma, Wmak = need(li + 3, li + 2)
                zak = [("za", c) for c in range(8)]
                for c in range(8):
                    yp, ypk = nextpb()
                    gp_, gpk = nextpb()
                    for kc in range(8):
                        P.mm(yp[:], Wco[:, kc, c * 128:(c + 1) * 128], za[:, kc, :], kc == 0, kc == 7, [Wcok, ("za", kc)], [ypk], inc=(kc == 7))
                    for kc in range(8):
                        P.mm(gp_[:], Wma[:, kc, c * 128:(c + 1) * 128], hxT[:, kc, blk], kc == 0, kc == 7, [Wmak, hk], [gpk], inc=(kc == 7))
                    t_, tk_ = nexttA()
                    P.act(t_[:], gp_[:], AF.Sigmoid, [gpk], [tk_])
                    P.tt("vector", big32[:, c, :], yp[:], t_[:], ALU.mult, [ypk, tk_], [("big32", c)])
                Wrg_, Wrgk = need(li + 4, li + 4)
                Wro, Wrok = need(li + 5, li + 4)
                for c in range(8):
                    hb_ = hsb[c % 2]
                    hbk = "hsb%d" % (c % 2)
                    P.dma("sync", hb_[:], hs_scr[c, :, blk], [("hs", c, j)], [hbk], hbk)
                    rp, rpk = nextpb()
                    for kc in range(8):
                        P.mm(rp[:], Wrg_[:, kc, c * 128:(c + 1) * 128], hxT[:, kc, blk], kc == 0, kc == 7, [Wrgk, hk], [rpk], inc=(kc == 7))
                    t_, tk_ = nexttA()
                    P.act(t_[:], rp[:], AF.Gelu_apprx_tanh, [rpk], [tk_])
                    P.tt("vector", zb[:, c, :], t_[:], hb_[:], ALU.mult, [tk_, hbk], [("zb", c)])
                if j == 0:
                    dump(zb[:, 0, :], [("zb", 0)])
                    dump(zb[:, 1, :], [("zb", 1)])
                    dump(big32[:, 0, :], [("big32", 0)])
                Wmb, Wmbk = need(li + 6, li + 5)
                for c in range(8):
                    yp, ypk = nextpb()
                    gp_, gpk = nextpb()
                    for kc in range(8):
                        P.mm(yp[:], Wro[:, kc, c * 128:(c + 1) * 128], zb[:, kc, :], kc == 0, kc == 7, [Wrok, ("zb", kc)], [ypk], inc=(kc == 7))
                    for kc in range(8):
                        P.mm(gp_[:], Wmb[:, kc, c * 128:(c + 1) * 128], hxT[:, kc, blk], kc == 0, kc == 7, [Wmbk, hk], [gpk], inc=(kc == 7))
                    t_, tk_ = nexttA()
                    P.act(t_[:], gp_[:], AF.Sigmoid, [gpk], [tk_])
                    P.tt("vector", t_[:], yp[:], t_[:], ALU.mult, [ypk, tk_], [tk_])
                    P.tt("vector", za[:, c, :], big32[:, c, :], t_[:], ALU.add, [("big32", c), tk_] + zak, [("za", c)])
                if j == 0:
                    dump(za[:, 0, :], [("za", 0)])
                    dump(za[:, 1, :], [("za", 1)])
                Wo, Wok = need(li + 7, li + 7)
                li += 8
                def stA(t, j=j, Wo=Wo, Wok=Wok):
                    tile_i = j * 4 + t
                    xb = x1t[tile_i % 2]
                    xbk = "x1t%d" % (tile_i % 2)
                    ops_ = []
                    for half in range(2):
                        op_, opk = nextpb()
                        ops_.append((op_, opk))
                        for kc in range(8):
                            P.mm(op_[:], za[:, kc, t * 128:(t + 1) * 128], Wo[:, kc, half * 512:(half + 1) * 512], kc == 0, kc == 7,
                                 [Wok, ("za", kc)], [opk], inc=(kc == 7))
                        P.act(junk16[:, 0:512], op_[:], AF.Square, [opk], ["junk16", ("ssB", half)], accum=small[:, 8 + half:9 + half])
                    P.tt("vector", small[:, 10:11], small[:, 8:9], small[:, 9:10], ALU.add, [("ssB", 0), ("ssB", 1)], ["ssB2"])
                    P.act(small[:, 11:12], small[:, 10:11], AF.Ln, ["ssB2"], ["sqB"], scale=1.0 / 1024, bias=EPS)
                    P.act(small[:, 12:13], small[:, 11:12], AF.Exp, ["sqB"], ["rstdB"], scale=-0.5)
                    xin = xt[tile_i % 2]
                    xink = "xt%d" % (tile_i % 2)
                    P.dma("sync", xin[:], xs[tile_i * 128:(tile_i + 1) * 128, :], (), [xink], xink)
                    for half in range(2):
                        op_, opk = ops_[half]
                        hs_ = slice(half * 512, (half + 1) * 512)
                        P.stt(xb[:, hs_], op_[:], small[:, 12:13], gvec1[:, hs_], ALU.mult, ALU.mult, [opk, "rstdB", "gvec1"], [xbk])
                    P.tt("vector", xb[:], xb[:], xin[:], ALU.add, [xbk, xink], [xbk])
                    P.dma("sync", out[tile_i * 128:(tile_i + 1) * 128, :], xb[:], [xbk], [("out", tile_i)], "o%d" % (tile_i % 2))
                    return norm_pre(None, xb, xbk)

                def stB(t, xn, j=j):
                    tile_i = j * 4 + t
                    norm_post(xn, 4, (hxT[:, :, tile_i * 128:(tile_i + 1) * 128], None), [("hx2", tile_i), ("hxT", j)], tbB, "tbB")

                def stC(t, j=j):
                    tile_i = j * 4 + t
                    lp, lpk = nextpb()
                    for kc in range(8):
                        P.mm(lp[:, 0:64], hxT[:, kc, tile_i * 128:(tile_i + 1) * 128], wrt16[:, kc, :], kc == 0, kc == 7,
                             [("hx2", tile_i), "wrt16"], [lpk], inc=(kc == 7))
                    sc_, sel_, eq_, t64 = tk[:, 0, :], tk[:, 1, :], tk[:, 2, :], tk[:, 3, :]
                    m1, m2, gs_, top8, gm_, pen = tk[:, 4, 0:8], tk[:, 4, 8:16], tk[:, 4, 16:24], tk[:, 4, 24:32], tk[:, 4, 32:40], tk[:, 4, 40:48]
                    K = ["tk"]
                    P.act(sc_, lp[:, 0:64], AF.Sigmoid, [lpk], K)
                    P.tt("vector", sel_, sc_, rbias_sb[:], ALU.add, K + ["rbias"], K)
                    s3 = sel_.rearrange("p (g e) -> p g e", e=8)
                    P.op("vector", lambda e, s3=s3, m1=m1: e.tensor_reduce(out=m1, in_=s3, axis=AX.X, op=ALU.max), K, K)
                    P.tt("vector", eq_.rearrange("p (g e) -> p g e", e=8), s3, m1.unsqueeze(2).to_broadcast([128, 8, 8]), ALU.is_equal, K, K)
                    P.stt(t64, eq_, -1e30, sel_, ALU.mult, ALU.add, K, K)
                    P.op("vector", lambda e, t64=t64, m2=m2: e.tensor_reduce(out=m2, in_=t64.rearrange("p (g e) -> p g e", e=8), axis=AX.X, op=ALU.max), K, K)
                    P.tt("vector", gs_, m1, m2, ALU.add, K, K)
                    P.op("vector", lambda e, gs_=gs_, top8=top8: e.max(out=top8, in_=gs_), K, K)
                    P.ts("vector", gm_, gs_, top8[:, 3:4], None, ALU.is_ge, None, K, K)
                    P.ts("vector", pen, gm_, 1e30, -1e30, ALU.mult, ALU.add, K, K)
                    P.tt("vector", t64.rearrange("p (g e) -> p g e", e=8), s3, pen.unsqueeze(2).to_broadcast([128, 8, 8]), ALU.add, K, K)
                    P.op("vector", lambda e, t64=t64, top8=top8: e.max(out=top8, in_=t64), K, K)
                    P.ts("vector", eq_, t64, top8[:, 7:8], None, ALU.is_ge, None, K, K)
                    P.tt("vector", eq_, eq_, sc_, ALU.mult, K, K)
                    P.op("vector", lambda e, eq_=eq_, m1=m1: e.tensor_reduce(out=m1[:, 0:1], in_=eq_, axis=AX.X, op=ALU.add), K, K)
                    P.op("vector", lambda e, m1=m1: e.reciprocal(out=m1[:, 1:2], in_=m1[:, 0:1]), K, K)
                    P.ts("vector", tk[:, 5 + (tile_i % 2), :], eq_, m1[:, 1:2], 2.5, ALU.mult, ALU.mult, K, [("gate", tile_i % 2)])

                def stD(t, j=j):
                    tile_i = j * 4 + t
                    gp_, gpk = nextpb()
                    P.tp(gp_[0:64, 0:128], tk[:, 5 + (tile_i % 2), :], identf[:], [("gate", tile_i % 2), "identf"], [gpk], inc=True)
                    P.act(gT[0:64, tile_i * 128:(tile_i + 1) * 128], gp_[0:64, 0:128], AF.Copy, [gpk], ["gT"])

                def tail_gen(stA=stA, stB=stB, stC=stC, stD=stD):
                    xns = {}
                    for step in range(4 + 3):
                        if step < 4:
                            xns[step] = stA(step)
                            yield
                        if 0 <= step - 1 < 4:
                            stB(step - 1, xns[step - 1])
                            yield
                        if 0 <= step - 2 < 4:
                            stC(step - 2)
                            yield
                        if 0 <= step - 3 < 4:
                            stD(step - 3)
                            yield
                pending_tail[0] = tail_gen()
            for _ in pending_tail[0]:
                pass
            P.dma("sync", gscr[0:65, :], gT[0:65, :], ["gT"], ["gscr"], "gsw")
            if dbg:
                P.op("sync", lambda e: None, ["gscr", "dbgo"] + [("out", i) for i in range(16)], (), inc=False)
                P.pend["sync"] = []
            P.run()
        if stop == "B":
            return nc

        with ExitStack() as ec:
            NE = 4
            ering = [sb(ec, "E%d" % i, [128, 6144], BF16) for i in range(NE)]
            acc = sb(ec, "acc", [128, 16, 1024], F32)
            hbuf = [[sb(ec, "h%d%d" % (p_, e_), [128, 2, 512], BF16) for e_ in range(2)] for p_ in range(2)]
            gbc = [sb(ec, "gbc%d" % i, [128, 512], BF16) for i in range(4)]
            s_sb = [sb(ec, "ssb%d" % i, [128, 512], BF16) for i in range(3)]
            su_sb = [sb(ec, "susb%d" % i, [128, 512], BF16) for i in range(3)]
            fo = [sb(ec, "fo%d" % i, [128, 1024], F32) for i in range(2)]
            gu = [ps(ec, "gu%d" % i, [128, 512], F32) for i in range(4)]
            ob = [ps(ec, "ob%d" % i, [128, 512], F32) for i in range(4)]

            def eload(e):
                s = e % NE
                P.dma("gpsimd", ering[s][:, 0:2048].rearrange("p (k n) -> p k n", n=256), wg[e].rearrange("(k p) n -> p k n", p=128), (), ["Eg%d" % s], "Eg%d" % s)
                P.dma("gpsimd", ering[s][:, 2048:4096].rearrange("p (k n) -> p k n", n=256), wu[e].rearrange("(k p) n -> p k n", p=128), (), ["Eu%d" % s], "Eu%d" % s)
                P.dma("gpsimd", ering[s][:, 4096:6144].rearrange("p (k n) -> p k n", n=1024), wd[e].rearrange("(k p) n -> p k n", p=128), (), ["Ed%d" % s], "Ed%d" % s)

            groups = [(2 * g, 2 * g + 1) for g in range(32)] + [(64,)]
            for e in groups[0] + groups[1]:
                eload(e)
            gbi = [0]
            gui = [0]
            si = [0]
            obi = [0]
            pend_d = [None]

            def do_down(gi_, grp, b, par):
                for t in range(4):
                    tile_i = b * 4 + t
                    for half in range(2):
                        o_i = obi[0] % 4
                        obi[0] += 1
                        op_, opk = ob[o_i], "ob%d" % o_i
                        n = len(grp) * 2
                        i = 0
                        for ei, e in enumerate(grp):
                            s = e % NE
                            wdv = ering[s][:, 4096:6144].rearrange("p (k n) -> p k n", n=1024)
                            for c in range(2):
                                P.mm(op_[:], hbuf[par][ei][:, c, t * 128:(t + 1) * 128], wdv[:, c, half * 512:(half + 1) * 512],
                                     i == 0, i == n - 1, ["Ed%d" % s, ("h", par, ei)], [opk], inc=(i == n - 1))
                                i += 1
                        dst = acc[:, tile_i, half * 512:(half + 1) * 512]
                        ak = ("acc", tile_i)
                        if gi_ == 0:
                            P.copy("vector", dst, op_[:], [opk], [ak])
                        else:
                            P.tt("vector", dst, dst, op_[:], ALU.add, [opk, ak], [ak])

            steps = [(gi_, grp, b) for gi_, grp in enumerate(groups) for b in range(4)]
            gbmap = {}

            def gbc_load(si_):
                gi_, grp, b = steps[si_]
                for ei, e in enumerate(grp):
                    gb_i = gbi[0] % 4
                    gbi[0] += 1
                    gk = "gbc%d" % gb_i
                    gbmap[(si_, ei)] = (gb_i, gk)
                    P.dma("gpsimd", gbc[gb_i][:].unsqueeze(1), gscr[e:e + 1, b * 512:(b + 1) * 512].partition_broadcast(128),
                          ["gscr"], [gk], gk)
            gbc_load(0)
            for si_, (gi_, grp, b) in enumerate(steps):
                par = si_ % 2
                blk = slice(b * 512, (b + 1) * 512)
                for ei, e in enumerate(grp):
                    s = e % NE
                    gb_i, gk = gbmap[(si_, ei)]
                    wgv = ering[s][:, 0:2048].rearrange("p (k n) -> p k n", n=256)
                    wuv = ering[s][:, 2048:4096].rearrange("p (k n) -> p k n", n=256)
                    for c in range(2):
                        g_i = gui[0] % 2
                        gui[0] += 1
                        gp_, gpk = gu[2 * g_i], "gu%d" % (2 * g_i)
                        up_, upk = gu[2 * g_i + 1], "gu%d" % (2 * g_i + 1)
                        for (pp, ppk, wv, wk_) in ((gp_, gpk, wgv, "Eg%d" % s), (up_, upk, wuv, "Eu%d" % s)):
                            for kc in range(8):
                                P.mm(pp[:], wv[:, kc, c * 128:(c + 1) * 128], hxT[:, kc, blk], kc == 0, kc == 7,
                                     [wk_] + [("hx2", b * 4 + t_) for t_ in range(4)], [ppk], inc=(kc == 7))
                        s_i = si[0] % 3
                        si[0] += 1
                        sk, suk = "ssb%d" % s_i, "susb%d" % s_i
                        P.act(s_sb[s_i][:], gp_[:], AF.Silu, [gpk], [sk])
                        P.tt("vector", su_sb[s_i][:], up_[:], s_sb[s_i][:], ALU.mult, [upk, sk], [suk])
                        P.tt("vector", hbuf[par][ei][:, c, :], su_sb[s_i][:], gbc[gb_i][:], ALU.mult, [suk, gk], [("h", par, ei)])
                if si_ + 1 < len(steps):
                    gbc_load(si_ + 1)
                if pend_d[0] is not None:
                    do_down(*pend_d[0])
                    pg = pend_d[0][0]
                    if pend_d[0][2] == 3 and pg + 2 < len(groups):
                        for e in groups[pg + 2]:
                            eload(e)
                pend_d[0] = (gi_, grp, b, par)
            do_down(*pend_d[0])
            for tile_i in range(16):
                xin = xt[tile_i % 2]
                xink = "xt%d" % (tile_i % 2)
                P.dma("sync", xin[:], out[tile_i * 128:(tile_i + 1) * 128, :], [("out", tile_i)], [xink], xink)
                ak = ("acc", tile_i)
                P.act(junk16[:], acc[:, tile_i, :], AF.Square, [ak], ["junk16", "ssC"], accum=small[:, 16:17])
                P.act(small[:, 17:18], small[:, 16:17], AF.Sqrt, ["ssC"], ["sqC"], scale=1.0 / 1024, bias=EPS)
                P.op("vector", lambda e: e.reciprocal(out=small[:, 18:19], in_=small[:, 17:18]), ["sqC"], ["rstdC"])
                f_ = fo[tile_i % 2]
                fk = "fo%d" % (tile_i % 2)
                P.stt(f_[:], acc[:, tile_i, :], small[:, 18:19], gvec2[:], ALU.mult, ALU.mult, [ak, "rstdC", "gvec2"], [fk])
                P.tt("vector", f_[:], f_[:], xin[:], ALU.add, [fk, xink], [fk])
                P.dma("sync", out[tile_i * 128:(tile_i + 1) * 128, :], f_[:], [fk], [("out", tile_i)], "o%d" % (tile_i % 2))
            P.op("sync", lambda e: None, [("out", i) for i in range(16)], (), inc=False)
            P.pend["sync"] = []
            P.run()
    return nc


def _prep_inputs(I, cid):
    b = cid // 2
    odd = cid % 2 == 1
    f = np.float32
    x = I['x'][b]
    ctx = I['ctx'][b]
    if odd:
        x = x[::-1]
        ctx = ctx[::-1]
    dP, dQ = (1, 0) if odd else (0, 1)
    w_sc = I['w_sc'][0]
    w_dw = I['w_dw'][0]
    bm = I['b_mod'][0].reshape(6, 1024)
    R = {
        'pre1_g': I['pre1_g'][0], 'pre2_g': I['pre2_g'][0], 'b_dw': I['b_dw'][0],
        'ln_g': I['ln_conv_g'][0], 'ln_b': I['ln_conv_b'][0],
        'bscP': I['b_sc'][0, dP], 'bscQ': I['b_sc'][0, dQ],
        'braP': I['b_rg_a'][0, dP], 'braQ': I['b_rg_a'][0, dQ],
        'brxP': I['b_rg_x'][0, dP], 'brxQ': I['b_rg_x'][0, dQ],
        'lamP': I['lru_lambda'][0, dP], 'lamQ': I['lru_lambda'][0, dQ],
        'bm_sh1': bm[0], 'bm_sc1': bm[1], 'bm_sh2': bm[3], 'bm_sc2': bm[4],
    }
    for i in range(31):
        R['dw%d' % i] = w_dw[30 - i] if odd else w_dw[i]
    for i in range(4):
        R['scP%d' % i] = w_sc[dP][3 - i] if odd else w_sc[dP][i]
        R['scQ%d' % i] = w_sc[dQ][3 - i] if odd else w_sc[dQ][i]
    V = np.stack([R[n] for n in VROWS]).astype(f)
    vecs = np.ascontiguousarray(V.reshape(len(VROWS), 8, 128).transpose(2, 0, 1).reshape(128, NV))
    cvT = np.ascontiguousarray(np.concatenate([I['c'][b].reshape(8, 128).T, I['c_ctx'].reshape(8, 128).T], axis=1)).astype(f)
    rows = np.stack([I['post1_g'][0], bm[2], I['post2_g'][0], bm[5]]).astype(f)
    w_rg = np.concatenate([I['w_rg_a'][0, dP], I['w_rg_x'][0, dP], I['w_rg_a'][0, dQ], I['w_rg_x'][0, dQ]], axis=0)
    return {
        'xs': np.ascontiguousarray(x, f), 'ctxs': np.ascontiguousarray(ctx, f), 'cvT': cvT, 'vecs': vecs, 'rows': rows,
        'rbias': np.ascontiguousarray(I['router_bias'][0].reshape(1, 64), f),
        'ident': np.eye(128, dtype=f),
        'w_rg': np.ascontiguousarray(w_rg, f),
    }


def kernel(**inputs):
    I = {k: np.asarray(v) for k, v in inputs.items()}
    shared = {
        'w_mod': np.ascontiguousarray(I['w_mod'][0], np.float32),
        'w_in': np.ascontiguousarray(I['w_in'][0], np.float32),
        'w_co': np.ascontiguousarray(I['w_conv_out'][0], np.float32),
        'w_ro': np.ascontiguousarray(I['w_rnn_out'][0], np.float32),
        'w_o': np.ascontiguousarray(I['w_out'][0], np.float32),
        'w_rt': np.ascontiguousarray(I['w_router'][0], np.float32),
        'wg': np.ascontiguousarray(np.concatenate([I['w_e_gate'][0], I['w_s_gate']], axis=0), np.float32),
        'wu': np.ascontiguousarray(np.concatenate([I['w_e_up'][0], I['w_s_up']], axis=0), np.float32),
        'wd': np.ascontiguousarray(np.concatenate([I['w_e_down'][0], I['w_s_down']], axis=0), np.float32),
    }
    in_maps = []
    for cid in range(8):
        m = dict(shared)
        m.update(_prep_inputs(I, cid))
        in_maps.append(m)
    nc = build()
    res = run_bass_kernel_spmd(nc, in_maps, core_ids=list(range(8)))
    outp = np.empty((4, 4096, 1024), np.float32)
    for cid in range(8):
        o = res.results[cid]["out"]
        b = cid // 2
        if cid % 2 == 0:
            outp[b, 0:2048] = o
        else:
            outp[b, 2048:4096] = o[::-1]
    return outp
```

```python
import numpy as np
from contextlib import ExitStack
import concourse.bass as bass
import concourse.mybir as mybir
from concourse.bass_utils import run_bass_kernel_spmd

F32 = mybir.dt.float32
BF16 = mybir.dt.bfloat16
AF = mybir.ActivationFunctionType
ALU = mybir.AluOpType
AX = mybir.AxisListType
EPS = 1e-6

VROWS = (['pre1_g', 'pre2_g', 'b_dw', 'ln_g', 'ln_b'] + ['dw%d' % i for i in range(31)]
         + ['scP%d' % i for i in range(4)] + ['scQ%d' % i for i in range(4)]
         + ['bscP', 'bscQ', 'braP', 'braQ', 'brxP', 'brxQ', 'lamP', 'lamQ',
            'bm_sh1', 'bm_sc1', 'bm_sh2', 'bm_sc2'])
VIDX = {n: i for i, n in enumerate(VROWS)}
NV = len(VROWS) * 8
NEXP = 65


class Prog:
    def __init__(self, nc, es):
        self.nc = nc
        self.es = es
        self.engs = ["tensor", "vector", "scalar", "gpsimd", "sync"]
        self.sem = {}
        for e in self.engs:
            self.sem["E_" + e] = es.enter_context(nc.semaphore("E_" + e))
        self.cnt = {e: 0 for e in self.engs}
        self.dcnt = {}
        self.ops = {e: [] for e in self.engs}
        self.waited = {e: {} for e in self.engs}
        self.lastw = {}
        self.rd = {}
        self.pend = {e: [] for e in self.engs}

    def _deps(self, eng, reads, writes):
        deps = {}

        def add(st):
            if st is None:
                return
            s, v = st
            if eng == "tensor" and s == "E_tensor":
                return
            if deps.get(s, 0) < v:
                deps[s] = v
        for r in reads:
            add(self.lastw.get(r))
        for w in writes:
            add(self.lastw.get(w))
            for s, v in self.rd.get(w, {}).items():
                add((s, v))
        waits = []
        wd = self.waited[eng]
        for s, v in deps.items():
            if wd.get(s, 0) < v:
                wd[s] = v
                waits.append((s, v))
        return waits

    def _stamp(self, st, reads, writes):
        s, v = st
        for r in reads:
            d = self.rd.setdefault(r, {})
            if d.get(s, 0) < v:
                d[s] = v
        for w in writes:
            self.lastw[w] = st
            self.rd[w] = {}

    def op(self, eng, fn, reads=(), writes=(), inc=True, dma=None):
        reads = tuple(reads)
        writes = tuple(writes)
        for e2 in self.engs:
            if e2 != eng and self.pend[e2]:
                for (pr, pw) in self.pend[e2]:
                    for k in reads + writes:
                        assert k not in pw, ("pending write hazard", k)
                    for k in writes:
                        assert k not in pr, ("pending read hazard", k)
        waits = self._deps(eng, reads, writes)
        if dma is not None:
            if dma not in self.sem:
                self.sem[dma] = self.es.enter_context(self.nc.semaphore(dma))
                self.dcnt[dma] = 0
            self.dcnt[dma] += 16
            st = (dma, self.dcnt[dma])
            self.ops[eng].append((waits, fn, ("dma", dma)))
            self._stamp(st, reads, writes)
        elif inc:
            self.cnt[eng] += 1
            st = ("E_" + eng, self.cnt[eng])
            self.ops[eng].append((waits, fn, ("inc", "E_" + eng)))
            for (r, w) in self.pend[eng]:
                self._stamp(st, r, w)
            self.pend[eng] = []
            self._stamp(st, reads, writes)
        else:
            self.ops[eng].append((waits, fn, None))
            self.pend[eng].append((reads, writes))

    def run(self):
        nc = self.nc
        for e in self.engs:
            assert not self.pend[e], e
        with nc.Block() as block:
            for e in self.engs:
                ops = self.ops[e]

                def body(eng, ops=ops):
                    for waits, fn, post in ops:
                        for s, v in waits:
                            eng.wait_ge(self.sem[s], v)
                        ins = fn(eng)
                        if post is not None:
                            kind, s = post
                            ins.then_inc(self.sem[s], 16 if kind == "dma" else 1)
                getattr(block, e)(body)
        self.ops = {e: [] for e in self.engs}

    def mm(self, out, lhsT, rhs, start, stop, R, W, inc):
        self.op("tensor", lambda e: e.matmul(out, lhsT=lhsT, rhs=rhs, start=start, stop=stop), R, W, inc=inc)

    def tp(self, out, in_, ident, R, W, inc):
        self.op("tensor", lambda e: e.transpose(out, in_, ident), R, W, inc=inc)

    def act(self, out, in_, func, R, W, bias=None, scale=None, accum=None):
        kw = {}
        if bias is not None:
            kw["bias"] = bias
        if scale is not None:
            kw["scale"] = scale
        if accum is not None:
            kw["accum_out"] = accum
        self.op("scalar", lambda e: e.activation(out=out, in_=in_, func=func, **kw), R, W)

    def tt(self, eng, out, in0, in1, op, R, W):
        self.op(eng, lambda e: e.tensor_tensor(out=out, in0=in0, in1=in1, op=op), R, W)

    def ts(self, eng, out, in0, s1, s2, op0, op1, R, W):
        if op1 is None:
            self.op(eng, lambda e: e.tensor_scalar(out=out, in0=in0, scalar1=s1, scalar2=None, op0=op0), R, W)
        else:
            self.op(eng, lambda e: e.tensor_scalar(out=out, in0=in0, scalar1=s1, scalar2=s2, op0=op0, op1=op1), R, W)

    def stt(self, out, in0, scalar, in1, op0, op1, R, W):
        self.op("vector", lambda e: e.scalar_tensor_tensor(out=out, in0=in0, scalar=scalar, in1=in1, op0=op0, op1=op1), R, W)

    def copy(self, eng, out, in_, R, W):
        self.op(eng, lambda e: e.tensor_copy(out=out, in_=in_), R, W)

    def memset(self, eng, ap, val, W):
        self.op(eng, lambda e: e.memset(ap, val), (), W)

    def dma(self, eng, out, in_, R, W, sem, **kw):
        self.op(eng, lambda e: e.dma_start(out=out, in_=in_, **kw), R, W, dma=sem)


def build(dbg=False, stop=None):
    nc = bass.Bass("TRN2", target_bir_lowering=False)

    def din(name, shape):
        return nc.dram_tensor(name, shape, F32, kind="ExternalInput").ap()
    xs = din("xs", [4096, 1024])
    ctxs = din("ctxs", [256, 1024])
    cvT = din("cvT", [128, 16])
    vecs = din("vecs", [128, NV])
    rows = din("rows", [4, 1024])
    rbias = din("rbias", [1, 64])
    identd = din("ident", [128, 128])
    w_mod = din("w_mod", [1024, 6144])
    w_in = din("w_in", [1024, 6144])
    w_rg = din("w_rg", [16, 256, 256])
    w_co = din("w_co", [1024, 1024])
    w_ro = din("w_ro", [1024, 1024])
    w_o = din("w_o", [1024, 1024])
    w_rt = din("w_rt", [1024, 64])
    wg = din("wg", [NEXP, 1024, 256])
    wu = din("wu", [NEXP, 1024, 256])
    wd = din("wd", [NEXP, 256, 1024])
    out = nc.dram_tensor("out", [2048, 1024], F32, kind="ExternalOutput").ap()
    sk_ = "ExternalOutput" if dbg else "Internal"
    hs_scr = nc.dram_tensor("hs_scr", [8, 128, 2048], F32, kind=sk_).ap()
    gscr = nc.dram_tensor("gscr", [NEXP, 2048], F32, kind=sk_).ap()
    dbgo = nc.dram_tensor("dbgo", [128, 4096], F32, kind="ExternalOutput").ap() if dbg else None

    with ExitStack() as es:
        P = Prog(nc, es)

        def sb(stack, name, shape, dt):
            return stack.enter_context(nc.sbuf_tensor(name, shape, dt))

        def ps(stack, name, shape, dt):
            return stack.enter_context(nc.psum_tensor(name, shape, dt))

        vec = sb(es, "vec", [128, NV], F32)
        cv = sb(es, "cv", [128, 16], F32)
        identf = sb(es, "identf", [128, 128], F32)
        identb = sb(es, "identb", [128, 128], BF16)
        onesf = sb(es, "onesf", [128, 128], F32)
        gvec1 = sb(es, "gvec1", [128, 1024], F32)
        gvec2 = sb(es, "gvec2", [128, 1024], F32)
        rbtmp = sb(es, "rbtmp", [128, 1024], F32)
        rbias_sb = sb(es, "rbias_sb", [128, 64], F32)
        mc = sb(es, "mc", [128, 6, 8], F32)
        clc = sb(es, "clc", [128, 2, 16], F32)
        hxT = sb(es, "hxT", [128, 8, 2048], BF16)
        wrt16 = sb(es, "wrt16", [128, 8, 64], BF16)
        small = sb(es, "small", [128, 64], F32)
        xn16s = [sb(es, "xn16_%d" % i, [128, 1024], BF16) for i in range(2)]
        xni = [0]
        junk16 = sb(es, "junk16", [128, 1024], BF16)
        modtmp = sb(es, "modtmp", [128, 8, 128], F32)
        xt = [sb(es, "xt%d" % i, [128, 1024], F32) for i in range(2)]

        def vcol(name, c=None):
            b = VIDX[name] * 8
            if c is None:
                return vec[:, b:b + 8]
            return vec[:, b + c:b + c + 1]

        cst_keys = ["vec", "cv", "identf", "gvec1", "rbtmp", "rbias"]
        P.dma("sync", vec[:], vecs, (), ["vec"], "cst")
        P.dma("sync", cv[:], cvT, (), ["cv"], "cst")
        P.dma("sync", identf[:], identd, (), ["identf"], "cst")
        P.dma("sync", gvec1[:].unsqueeze(1), rows[1:2, :].partition_broadcast(128), (), ["gvec1"], "cst")
        P.dma("sync", rbtmp[:].unsqueeze(1), rows[0:1, :].partition_broadcast(128), (), ["rbtmp"], "cst")
        P.dma("sync", rbias_sb[:].unsqueeze(1), rbias[0:1, :].partition_broadcast(128), (), ["rbias"], "cst")
        for k in cst_keys:
            P.lastw[k] = ("cst", P.dcnt["cst"])
        P.copy("vector", identb[:], identf[:], ["identf"], ["identb"])
        P.memset("vector", onesf[:], 1.0, ["onesf"])

        xt_i = [0]

        def norm_pre(src_dram, src_sb, src_key):
            if src_sb is None:
                b = xt_i[0] % 2
                xt_i[0] += 1
                src_sb = xt[b]
                src_key = "xt%d" % b
                P.dma("sync", src_sb[:], src_dram, (), [src_key], src_key)
            xb_ = xni[0] % 2
            xni[0] += 1
            xn16, xnk = xn16s[xb_], "xn16_%d" % xb_
            P.act(junk16[:], src_sb[:], AF.Square, [src_key], ["junk16", "ss"], accum=small[:, 0:1])
            P.act(small[:, 1:2], small[:, 0:1], AF.Ln, ["ss"], ["sq"], scale=1.0 / 1024, bias=EPS)
            P.act(small[:, 2:3], small[:, 1:2], AF.Exp, ["sq"], ["rstd"], scale=-0.5)
            P.act(xn16[:], src_sb[:], AF.Copy, [src_key, "rstd"], [xnk], scale=small[:, 2:3])
            return xn16, xnk

        def norm_post(xn, Aidx, dst_col, dst_key, tbank, tbkey):
            xn16, xnk = xn
            for c in range(8):
                P.tp(tbank[:, c * 128:(c + 1) * 128], xn16[:, c * 128:(c + 1) * 128], identb[:],
                     [xnk, "identb"], [tbkey], inc=(c == 7))
            tv = tbank[:].rearrange("p (c t) -> p c t", t=128)
            Ab = mc[:, Aidx, :].unsqueeze(2).to_broadcast([128, 8, 128])
            Bb = mc[:, Aidx + 1, :].unsqueeze(2).to_broadcast([128, 8, 128])
            P.tt("vector", modtmp[:], tv, Ab, ALU.mult, [tbkey, "mc"], ["modtmp"])
            dst, dkey = dst_col
            P.tt("vector", dst, modtmp[:], Bb, ALU.add, ["modtmp", "mc"], dst_key if isinstance(dst_key, list) else [dst_key])

        def norm_tile(src_dram, src_sb, src_key, Aidx, dst_col, dst_key, tbank, tbkey):
            xn = norm_pre(src_dram, src_sb, src_key)
            norm_post(xn, Aidx, dst_col, dst_key, tbank, tbkey)

        with ExitStack() as ea:
            S0 = sb(ea, "S0", [128, 8192], BF16)
            S1 = sb(ea, "S1", [128, 8192], BF16)
            dsc = sb(ea, "dsc", [128, 64, 128], BF16)
            sc32 = sb(ea, "sc32", [128, 16], F32)
            sc16 = sb(ea, "sc16", [128, 16], BF16)
            screp = sb(ea, "screp", [128, 8, 128], BF16)
            modfm = sb(ea, "modfm", [128, 64], F32)
            ltmp = sb(ea, "ltmp", [128, 3, 16], F32)
            hb = sb(ea, "hb", [128, 32], F32)
            hTt = sb(ea, "hTt", [128, 8, 512], BF16)
            u16s = [sb(ea, "u16_%d" % i, [128, 8, 518], BF16) for i in range(2)]
            svP = sb(ea, "svP", [128, 8, 3], BF16)
            svQ = sb(ea, "svQ", [128, 8, 3], BF16)
            v32 = sb(ea, "v32", [128, 8, 512], F32)
            v16 = sb(ea, "v16", [128, 8, 512], BF16)
            houts = [sb(ea, "hout%d" % i, [128, 512], F32) for i in range(4)]
            hoi = [0]
            gt = [[sb(ea, "g%s%d" % (n, i), [128, 512], F32) for n in "riam"] for i in range(4)]
            stt_ = sb(ea, "state", [128, 2, 8], F32)
            pb = [ps(ea, "pb%d" % i, [128, 512], F32) for i in range(6)]
            tb = [ps(ea, "tb%d" % i, [128, 1024], BF16) for i in range(2)]
            pbi = [0]

            def nextpb():
                i = pbi[0] % 6
                pbi[0] += 1
                return pb[i], "pb%d" % i
            tbi = [0]

            def nexttb():
                i = tbi[0] % 2
                tbi[0] += 1
                return tb[i], "tb%d" % i

            S = [S0, S1]

            def slotv(i):
                return S[i][:].rearrange("p (k n) -> p k n", n=1024)

            P.act(sc32[:], cv[:], AF.Silu, ["cv"], ["sc32"])
            P.copy("vector", sc16[:], sc32[:], ["sc32"], ["sc16"])
            P.copy("vector", screp[:], sc32[:, 0:8].unsqueeze(2).to_broadcast([128, 8, 128]), ["sc32"], ["screp"])
            modps, modk = nextpb()
            fm_i = 0
            for q in range(6):
                si = q % 2
                sv = slotv(si)
                P.dma("gpsimd", sv, w_mod[:, q * 1024:(q + 1) * 1024].rearrange("(k p) n -> p k n", p=128),
                      (), ["S%d" % si], "S%d" % si)
                if q in (0, 1, 3, 4):
                    for j in range(8):
                        col = (fm_i * 8 + j) * 2
                        for kc in range(8):
                            P.mm(modps[:, col:col + 2], sv[:, kc, j * 128:(j + 1) * 128], sc16[:, kc:16:8],
                                 kc == 0, kc == 7, ["S%d" % si, "sc16"], [modk], inc=(j == 7 and kc == 7))
                    fm_i += 1
                else:
                    gv = gvec1 if q == 2 else gvec2
                    gk = "gvec1" if q == 2 else "gvec2"
                    if q == 5:
                        P.dma("sync", gvec2[:].unsqueeze(1), rows[3:4, :].partition_broadcast(128), (), ["gvec2"], "cst2")
                        P.dma("sync", rbtmp[:].unsqueeze(1), rows[2:3, :].partition_broadcast(128), (), ["rbtmp"], "cst3")
                    for half in range(2):
                        gp, gpk = nextpb()
                        for kc in range(8):
                            P.mm(gp[:], screp[:, kc, :], sv[:, kc, half * 512:(half + 1) * 512], kc == 0, kc == 7,
                                 ["S%d" % si, "screp"], [gpk], inc=(kc == 7))
                        hs_ = slice(half * 512, (half + 1) * 512)
                        P.tt("vector", gv[:, hs_], gp[:], gv[:, hs_], ALU.add, [gpk, gk], [gk])
                        P.tt("vector", gv[:, hs_], gv[:, hs_], rbtmp[:, hs_], ALU.mult, [gk, "rbtmp"], [gk])
            P.act(modfm[:], modps[:, 0:64], AF.Copy, [modk], ["modfm"])
            mv = modfm[:].rearrange("p (q j t) -> p q j t", q=4, j=8)
            P.tt("vector", mc[:, 1, :], mv[:, 0, :, 0], vcol("bm_sh1"), ALU.add, ["modfm", "vec"], ["mc"])
            P.tt("vector", mc[:, 3, :], mv[:, 0, :, 1], vcol("bm_sh1"), ALU.add, ["modfm", "vec"], ["mc"])
            P.tt("vector", mc[:, 5, :], mv[:, 2, :, 0], vcol("bm_sh2"), ALU.add, ["modfm", "vec"], ["mc"])
            for (dst, qi, t, bmn, gn) in ((0, 1, 0, "bm_sc1", "pre1_g"), (2, 1, 1, "bm_sc1", "pre1_g"), (4, 3, 0, "bm_sc2", "pre2_g")):
                P.tt("vector", mc[:, dst, :], mv[:, qi, :, t], vcol(bmn), ALU.add, ["modfm", "vec", "mc"], ["mc"])
                P.stt(mc[:, dst, :], mc[:, dst, :], 1.0, vcol(gn), ALU.add, ALU.mult, ["mc", "vec"], ["mc"])
            lb = VIDX["lamP"] * 8
            P.act(ltmp[:, 0, :], vec[:, lb:lb + 16], AF.Exp, ["vec"], ["ltmp"], scale=-1.0)
            P.ts("vector", ltmp[:, 1, :], ltmp[:, 0, :], -1.0 / 3, 0.5, ALU.mult, ALU.add, ["ltmp"], ["ltmp"])
            P.tt("vector", ltmp[:, 1, :], ltmp[:, 1, :], ltmp[:, 0, :], ALU.mult, ["ltmp"], ["ltmp"])
            P.ts("vector", ltmp[:, 1, :], ltmp[:, 1, :], -1.0, 1.0, ALU.mult, ALU.add, ["ltmp"], ["ltmp"])
            P.tt("vector", ltmp[:, 2, :], ltmp[:, 1, :], ltmp[:, 0, :], ALU.mult, ["ltmp"], ["ltmp"])
            P.ts("vector", clc[:, 0, :], ltmp[:, 2, :], -8.0, None, ALU.mult, None, ["ltmp"], ["clc"])
            P.ts("vector", clc[:, 1, :], ltmp[:, 2, :], -16.0, None, ALU.mult, None, ["ltmp"], ["clc"])
            bb0 = VIDX["braP"] * 8
            P.ts("vector", hb[:], vec[:, bb0:bb0 + 32], -1.0, None, ALU.mult, None, ["vec"], ["hb"])
            sb0 = VIDX["scP0"] * 8
            P.tt("vector", dsc[:], identb[:].unsqueeze(1).to_broadcast([128, 64, 128]),
                 vec[:, sb0:sb0 + 64].unsqueeze(2).to_broadcast([128, 64, 128]), ALU.mult, ["identb", "vec"],
                 [("dsc", i) for i in range(64)])
            wxv = slotv(0)
            P.dma("gpsimd", wxv, w_in[:, 2048:3072].rearrange("(k p) n -> p k n", p=128), (), ["S0"], "S0")
            wrgv = S1[:].rearrange("p (m n) -> p m n", n=256)
            P.dma("gpsimd", wrgv, w_rg.rearrange("m (kk p) n -> p (m kk) n", p=128), (), ["S1"], "S1")
            P.dma("gpsimd", wrt16[:], w_rt.rearrange("(k p) n -> p k n", p=128), (), ["wrt16"], "wrt")
            P.memset("vector", stt_[:], 0.0, ["stP", "stQ"])

            gset = [0]

            def stage1(src_rows, ntile, Aidx, hT, hT_c0, hT_key, do_norm, haloL, haloR, ub):
                T = ntile * 128
                u16 = u16s[ub]
                if do_norm:
                    for t in range(ntile):
                        tbk, tbkey = nexttb()
                        norm_tile(src_rows[t * 128:(t + 1) * 128, :], None, None, Aidx,
                                  (hT[:, :, hT_c0 + t * 128: hT_c0 + (t + 1) * 128], None), hT_key, tbk, tbkey)
                        yield
                for m in range(8):
                    up, upk = nextpb()
                    for kc in range(8):
                        P.mm(up[:, 0:T], wxv[:, kc, m * 128:(m + 1) * 128], hT[:, kc, hT_c0:hT_c0 + T], kc == 0, kc == 7,
                             ["S0", hT_key], [upk], inc=(kc == 7))
                    P.copy("vector", u16[:, m, 3:3 + T], up[:, 0:T], [upk], [("u16", ub, m)])
                    yield
                uk = [("u16", ub, m) for m in range(8)]
                if haloL == "zero":
                    P.memset("vector", u16[:, :, 0:3], 0.0, [("u16L", ub)])
                else:
                    P.copy("vector", u16[:, :, 0:3], svP[:], ["svP"], [("u16L", ub)])
                if haloR == "zero":
                    P.memset("vector", u16[:, :, 3 + T:6 + T], 0.0, [("u16R", ub)])
                else:
                    P.copy("vector", u16[:, :, 3 + T:6 + T], svQ[:], ["svQ"], [("u16R", ub)])
                P.copy("vector", svP[:], u16[:, :, T:T + 3], uk, ["svP"])
                P.copy("vector", svQ[:], u16[:, :, 3:6], uk, ["svQ"])
                yield

            def stage2(ntile, dirs, store, blk, ub, tick):
                T = ntile * 128
                u16 = u16s[ub]
                for d in dirs:
                    di = 0 if d == "P" else 1
                    hk = ("u16L", ub) if d == "P" else ("u16R", ub)
                    def conv(c, d=d, di=di, hk=hk):
                        cp, cpk = nextpb()
                        for j in range(4):
                            off = j if d == "P" else 3 + j
                            P.mm(cp[:, 0:T], dsc[:, (di * 4 + j) * 8 + c, :], u16[:, c, off:off + T], j == 0, j == 3,
                                 [("dsc", (di * 4 + j) * 8 + c), ("u16", ub, c), hk], [cpk], inc=(j == 3))
                        P.ts("vector", v32[:, c, 0:T], cp[:, 0:T], vcol("bsc" + d, c), None, ALU.add, None, [cpk, "vec"], [("v32", c)])
                        P.copy("vector", v16[:, c, 0:T], v32[:, c, 0:T], [("v32", c)], [("v16", c)])
                        tick()
                    def ph1(m, d=d, di=di):
                        h = m // 2
                        gs = gset[0] % 4
                        gset[0] += 1
                        gr, gi, ga, gm = gt[gs]
                        kr, ki, ka, km = [("g", gs, n) for n in "riam"]
                        rp, rpk = nextpb()
                        ip, ipk = nextpb()
                        for (pp, ppk, mat) in ((rp, rpk, di * 2), (ip, ipk, di * 2 + 1)):
                            for kk in range(2):
                                P.mm(pp[:, 0:T], wrgv[:, (mat * 4 + h) * 2 + kk, (m % 2) * 128:(m % 2 + 1) * 128],
                                     v16[:, 2 * h + kk, 0:T], kk == 0, kk == 1, ["S1", ("v16", 2 * h + kk)], [ppk], inc=(kk == 1))
                        P.act(gr[:, 0:T], rp[:, 0:T], AF.Exp, [rpk, "hb"], [kr], scale=-1.0, bias=hb[:, di * 8 + m:di * 8 + m + 1])
                        P.act(gi[:, 0:T], ip[:, 0:T], AF.Exp, [ipk, "hb"], [ki], scale=-1.0, bias=hb[:, 16 + di * 8 + m:16 + di * 8 + m + 1])
                        P.act(gr[:, 0:T], gr[:, 0:T], AF.Ln, [kr], [kr], scale=1.0, bias=1.0)
                        P.act(gi[:, 0:T], gi[:, 0:T], AF.Ln, [ki], [ki], scale=1.0, bias=1.0)
                        P.act(gr[:, 0:T], gr[:, 0:T], AF.Exp, [kr], [kr], scale=-1.0)
                        c0_ = clc[:, 0, di * 8 + m:di * 8 + m + 1]
                        P.act(ga[:, 0:T], gr[:, 0:T], AF.Exp, [kr, "clc"], [ka], scale=c0_)
                        P.tt("vector", gm[:, 0:T], ga[:, 0:T], ga[:, 0:T], ALU.mult, [ka], [km])
                        return gs

                    def ph2(m, gs, d=d, di=di):
                        gr, gi, ga, gm = gt[gs]
                        kr, ki, ka, km = [("g", gs, n) for n in "riam"]
                        P.act(gm[:, 0:T], gm[:, 0:T], AF.Ln, [km], [km], scale=-1.0, bias=1.0)
                        P.stt(gi[:, 0:T], gm[:, 0:T], 0.5, gi[:, 0:T], ALU.mult, ALU.subtract, [km, ki], [ki])

                    def ph3(m, gs, d=d, di=di):
                        gr, gi, ga, gm = gt[gs]
                        kr, ki, ka, km = [("g", gs, n) for n in "riam"]
                        gb, kb = gi, ki
                        P.act(gi[:, 0:T], gi[:, 0:T], AF.Exp, [ki], [ki])
                        P.tt("vector", gi[:, 0:T], gi[:, 0:T], v32[:, m, 0:T], ALU.mult, [ki, ("v32", m)], [ki])
                        stk = "st" + d
                        hi_ = hoi[0] % 4
                        hoi[0] += 1
                        ho, hok = houts[hi_], "hout%d" % hi_
                        if d == "P":
                            o_, a_, b_ = ho[:, 0:T], ga[:, 0:T], gb[:, 0:T]
                            last = ho[:, T - 1:T]
                        else:
                            o_, a_, b_ = ho[:, 0:T][:, ::-1], ga[:, 0:T][:, ::-1], gb[:, 0:T][:, ::-1]
                            last = ho[:, 0:1]
                        init = stt_[:, di, m:m + 1]
                        P.op("vector", lambda e, o_=o_, a_=a_, b_=b_, init=init: e.tensor_tensor_scan(
                            out=o_, data0=a_, data1=b_, initial=init, op0=ALU.mult, op1=ALU.add),
                            [ka, kb, (stk, m)], [hok])
                        P.copy("vector", stt_[:, di, m:m + 1], last, [hok], [(stk, m)])
                        if store == "write":
                            P.dma("sync", hs_scr[m, :, blk * 512:(blk + 1) * 512], ho[:], [hok],
                                  [("hs", m, blk)], "hsp%d" % hi_)
                        elif store == "accum":
                            P.dma("gpsimd", hs_scr[m, :, blk * 512:(blk + 1) * 512], ho[:], [hok],
                                  [("hs", m, blk)], "hsq%d" % hi_, accum_op=ALU.add)
                        tick()

                    q1 = None
                    q2 = None
                    conv(0)
                    conv(1)
                    for m in range(8 + 2):
                        if m < 8 and m % 2 == 0 and m + 2 < 8:
                            conv(m + 2)
                            conv(m + 3)
                        new1 = (m, ph1(m)) if m < 8 else None
                        if q1 is not None:
                            ph2(*q1)
                        if q2 is not None:
                            ph3(*q2)
                        q2 = q1
                        q1 = new1

            for m in range(8):
                P.lastw[("stP", m)] = P.lastw["stP"]
                P.lastw[("stQ", m)] = P.lastw["stQ"]
            passes = []
            passes.append(dict(src=ctxs, ntile=2, Aidx=2, hT=hTt, c0=0, hk="hTt", norm=True, dirs=["P", "Q"], hL="zero", hR="zero", store=None, blk=0))
            for j in range(4):
                passes.append(dict(src=xs[j * 512:(j + 1) * 512, :], ntile=4, Aidx=0, hT=hxT, c0=j * 512, hk=("hxT", j), norm=True, dirs=["P"],
                                   hL="zero" if j == 0 else "chain", hR="zero", store="write", blk=j))
            for j in (7, 6, 5, 4):
                passes.append(dict(src=xs[j * 512:(j + 1) * 512, :], ntile=4, Aidx=0, hT=hTt, c0=0, hk="hTt", norm=True, dirs=["Q"],
                                   hL="zero", hR="zero" if j == 7 else "chain", store=None, blk=j))
            for j in (3, 2, 1, 0):
                passes.append(dict(src=None, ntile=4, Aidx=0, hT=hxT, c0=j * 512, hk=("hxT", j), norm=False, dirs=["Q"],
                                   hL="zero", hR="chain", store="accum", blk=j))

            def mk1(k):
                p = passes[k]
                return stage1(p["src"], p["ntile"], p["Aidx"], p["hT"], p["c0"], p["hk"], p["norm"], p["hL"], p["hR"], k % 2)
            for _ in mk1(0):
                pass
            for k, p in enumerate(passes):
                gen = mk1(k + 1) if k + 1 < len(passes) else iter(())

                def tick(gen=gen):
                    next(gen, None)
                stage2(p["ntile"], p["dirs"], p["store"], p["blk"], k % 2, tick)
                for _ in gen:
                    pass
            if dbg:
                P.copy("vector", modtmp[:, 0:6, 0:8], mc[:], ["mc"], ["dbgt"])
                P.copy("vector", modtmp[:, 6:8, 0:8], stt_[:], [("stP", m) for m in range(8)] + [("stQ", m) for m in range(8)], ["dbgt"])
                P.dma("sync", dbgo[:, 0:1024], modtmp[:].rearrange("p a b -> p (a b)"), ["dbgt"], ["dbgo"], "dbg")
                P.dma("sync", dbgo[:, 1024:2048], gvec1[:], ["gvec1"], ["dbgo1"], "dbg1")
                P.dma("sync", dbgo[:, 2048:3072], gvec2[:], ["gvec2"], ["dbgo2"], "dbg2")
                P.op("sync", lambda e: None, ["dbgo", "dbgo1", "dbgo2"] + [("hs", m, j) for m in range(8) for j in range(4)], (), inc=False)
                P.pend["sync"] = []
            P.run()
        if stop == "A":
            return nc

        with ExitStack() as eb:
            NS = 3
            ring = [sb(eb, "R%d" % i, [128, 8, 1024], BF16) for i in range(NS)]
            ucp = sb(eb, "ucp", [128, 8, 8, 94], BF16)
            big32 = sb(eb, "big32", [128, 8, 512], F32)
            za = sb(eb, "za", [128, 8, 512], BF16)
            zb = sb(eb, "zb", [128, 8, 512], BF16)
            hsb = [sb(eb, "hsb%d" % i, [128, 512], F32) for i in range(2)]
            ddw2 = [sb(eb, "ddw%d" % i, [128, 31, 128], BF16) for i in range(2)]
            ddi = [0]
            tA = [sb(eb, "tA%d" % i, [128, 512], F32) for i in range(3)]
            mean_sb = sb(eb, "mean_sb", [128, 512], F32)
            rstd_sb = sb(eb, "rstd_sb", [128, 512], F32)
            x1t = [sb(eb, "x1t%d" % i, [128, 1024], F32) for i in range(2)]
            gT = sb(eb, "gT", [128, 2048], F32)
            tk = sb(eb, "tk", [128, 8, 64], F32)
            dbgst = sb(eb, "dbgst", [128, 512], F32) if dbg else None
            dslot = [0]

            def dump(ap, keys):
                if not dbg or dslot[0] >= 8:
                    return
                i = dslot[0]
                dslot[0] += 1
                P.copy("vector", dbgst[:], ap, keys, ["dbgst"])
                P.dma("sync", dbgo[:, i * 512:(i + 1) * 512], dbgst[:], ["dbgst"], ["dbgo"], "dbgB")
            pb = [ps(eb, "qb%d" % i, [128, 512], F32) for i in range(7)]
            tbB = ps(eb, "tbB", [128, 1024], BF16)
            pbi = [0]

            def nextpb():
                i = pbi[0] % 5
                pbi[0] += 1
                return pb[i], "qb%d" % i
            tai = [0]

            def nexttA():
                i = tai[0] % 3
                tai[0] += 1
                return tA[i], "tA%d" % i

            loads = []
            for j in range(4):
                loads += [(w_in, 0), (w_in, 1024), (w_co, 0), (w_in, 4096), (w_in, 3072), (w_ro, 0), (w_in, 5120), (w_o, 0)]
            issued = [0]

            def need(k, done):
                while issued[0] <= min(done - 1 + NS, len(loads) - 1):
                    i = issued[0]
                    w_, c0 = loads[i]
                    s = i % NS
                    P.dma("gpsimd", ring[s][:], w_[:, c0:c0 + 1024].rearrange("(k p) n -> p k n", p=128),
                          (), ["R%d" % s], "R%d" % s)
                    issued[0] += 1
                assert issued[0] > k
                return ring[k % NS], "R%d" % (k % NS)

            P.memset("vector", ucp[:], 0.0, [("ucp", c) for c in range(8)])
            P.memset("vector", gT[:], 1.0, ["gT"])
            dwb = VIDX["dw0"] * 8
            li = 0
            pending_tail = [iter(())]
            for j in range(4):
                blk = slice(j * 512, (j + 1) * 512)
                hk = ("hxT", j)
                Wa, Wak = need(li, li if j == 0 else li - 1)
                Wg, Wgk = need(li + 1, li if j == 0 else li - 1)
                for c in range(8):
                    ap_, apk = nextpb()
                    gp_, gpk = nextpb()
                    for (pp, ppk, W_, Wk_) in ((ap_, apk, Wa, Wak), (gp_, gpk, Wg, Wgk)):
                        for kc in range(8):
                            P.mm(pp[:], W_[:, kc, c * 128:(c + 1) * 128], hxT[:, kc, blk], kc == 0, kc == 7,
                                 [Wk_, hk], [ppk], inc=(kc == 7))
                    t_, tk_ = nexttA()
                    P.act(t_[:], gp_[:], AF.Sigmoid, [gpk], [tk_])
                    P.tt("vector", ucp[:, c, :, 15:79], ap_[:].rearrange("p (r t) -> p r t", t=64),
                         t_[:].rearrange("p (r t) -> p r t", t=64), ALU.mult, [apk, tk_], [("ucp", c)])
                    next(pending_tail[0], None)
                    next(pending_tail[0], None)
                for _ in pending_tail[0]:
                    pass
                mp, mpk = pb[5], "qb5"
                sp_, spk = pb[6], "qb6"
                for c in range(8):
                    ddw = ddw2[ddi[0] % 2]
                    ddk = "ddw%d" % (ddi[0] % 2)
                    ddi[0] += 1
                    P.tt("vector", ddw[:], identb[:].unsqueeze(1).to_broadcast([128, 31, 128]),
                         vec[:, dwb + c:dwb + 31 * 8:8].unsqueeze(2).to_broadcast([128, 31, 128]), ALU.mult,
                         ["identb", "vec"], [ddk])
                    cp, cpk = nextpb()
                    for tap in range(31):
                        P.mm(cp[:].rearrange("p (r t) -> p r t", t=64), ddw[:, tap, :], ucp[:, c, :, tap:tap + 64], tap == 0, tap == 30,
                             [ddk, ("ucp", c)], [cpk], inc=(tap == 30))
                    P.act(big32[:, c, :], cp[:], AF.Identity, [cpk, "vec"], [("big32", c)], bias=vcol("b_dw", c))
                    if j == 0 and c == 0:
                        dump(big32[:, 0, :], [("big32", 0)])
                    t_, tk_ = nexttA()
                    P.act(t_[:], cp[:], AF.Square, [cpk, "vec"], [tk_], bias=vcol("b_dw", c))
                    P.mm(mp[:], onesf[:], big32[:, c, :], c == 0, c == 7, ["onesf", ("big32", c)], [mpk], inc=(c == 7))
                    P.mm(sp_[:], onesf[:], t_[:], c == 0, c == 7, ["onesf", tk_], [spk], inc=(c == 7))
                P.ts("vector", mean_sb[:], mp[:], 1.0 / 1024, None, ALU.mult, None, [mpk], ["mean_sb"])
                P.tt("vector", rstd_sb[:], mean_sb[:], mean_sb[:], ALU.mult, ["mean_sb"], ["rstd_sb"])
                P.stt(rstd_sb[:], sp_[:], 1.0 / 1024, rstd_sb[:], ALU.mult, ALU.subtract, [spk, "rstd_sb"], ["rstd_sb"])
                P.act(rstd_sb[:], rstd_sb[:], AF.Sqrt, ["rstd_sb"], ["rstd_sb"], bias=EPS, scale=1.0)
                P.op("vector", lambda e: e.reciprocal(out=rstd_sb[:], in_=rstd_sb[:]), ["rstd_sb"], ["rstd_sb"])
                for c in range(8):
                    P.tt("vector", big32[:, c, :], big32[:, c, :], mean_sb[:], ALU.subtract, [("big32", c), "mean_sb"], [("big32", c)])
                    P.tt("vector", big32[:, c, :], big32[:, c, :], rstd_sb[:], ALU.mult, [("big32", c), "rstd_sb"], [("big32", c)])
                    P.act(za[:, c, :], big32[:, c, :], AF.Silu, [("big32", c), "vec"], [("za", c)],
                          scale=vcol("ln_g", c), bias=vcol("ln_b", c))
                if j == 0:
                    dump(za[:, 0, :], [("za", 0)])
                    dump(za[:, 1, :], [("za", 1)])
                Wco, Wcok = need(li + 2, li + 2)
                Wma, Wmak = need(li + 3, li + 2)
                zak = [("za", c) for c in range(8)]
                for c in range(8):
                    yp, ypk = nextpb()
                    gp_, gpk = nextpb()
                    for kc in range(8):
                        P.mm(yp[:], Wco[:, kc, c * 128:(c + 1) * 128], za[:, kc, :], kc == 0, kc == 7, [Wcok, ("za", kc)], [ypk], inc=(kc == 7))
                    for kc in range(8):
                        P.mm(gp_[:], Wma[:, kc, c * 128:(c + 1) * 128], hxT[:, kc, blk], kc == 0, kc == 7, [Wmak, hk], [gpk], inc=(kc == 7))
                    t_, tk_ = nexttA()
                    P.act(t_[:], gp_[:], AF.Sigmoid, [gpk], [tk_])
                    P.tt("vector", big32[:, c, :], yp[:], t_[:], ALU.mult, [ypk, tk_], [("big32", c)])
                Wrg_, Wrgk = need(li + 4, li + 4)
                Wro, Wrok = need(li + 5, li + 4)
                for c in range(8):
                    hb_ = hsb[c % 2]
                    hbk = "hsb%d" % (c % 2)
                    P.dma("sync", hb_[:], hs_scr[c, :, blk], [("hs", c, j)], [hbk], hbk)
                    rp, rpk = nextpb()
                    for kc in range(8):
                        P.mm(rp[:], Wrg_[:, kc, c * 128:(c + 1) * 128], hxT[:, kc, blk], kc == 0, kc == 7, [Wrgk, hk], [rpk], inc=(kc == 7))
                    t_, tk_ = nexttA()
                    P.act(t_[:], rp[:], AF.Gelu_apprx_tanh, [rpk], [tk_])
                    P.tt("vector", zb[:, c, :], t_[:], hb_[:], ALU.mult, [tk_, hbk], [("zb", c)])
                if j == 0:
                    dump(zb[:, 0, :], [("zb", 0)])
                    dump(zb[:, 1, :], [("zb", 1)])
                    dump(big32[:, 0, :], [("big32", 0)])
                Wmb, Wmbk = need(li + 6, li + 5)
                for c in range(8):
                    yp, ypk = nextpb()
                    gp_, gpk = nextpb()
                    for kc in range(8):
                        P.mm(yp[:], Wro[:, kc, c * 128:(c + 1) * 128], zb[:, kc, :], kc == 0, kc == 7, [Wrok, ("zb", kc)], [ypk], inc=(kc == 7))
                    for kc in range(8):
                        P.mm(gp_[:], Wmb[:, kc, c * 128:(c + 1) * 128], hxT[:, kc, blk], kc == 0, kc == 7, [Wmbk, hk], [gpk], inc=(kc == 7))
                    t_, tk_ = nexttA()
                    P.act(t_[:], gp_[:], AF.Sigmoid, [gpk], [tk_])
                    P.tt("vector", t_[:], yp[:], t_[:], ALU.mult, [ypk, tk_], [tk_])
                    P.tt("vector", za[:, c, :], big32[:, c, :], t_[:], ALU.add, [("big32", c), tk_] + zak, [("za", c)])
                if j == 0:
                    dump(za[:, 0, :], [("za", 0)])
                    dump(za[:, 1, :], [("za", 1)])
                Wo, Wok = need(li + 7, li + 7)
                li += 8
                def stA(t, j=j, Wo=Wo, Wok=Wok):
                    tile_i = j * 4 + t
                    xb = x1t[tile_i % 2]
                    xbk = "x1t%d" % (tile_i % 2)
                    ops_ = []
                    for half in range(2):
                        op_, opk = nextpb()
                        ops_.append((op_, opk))
                        for kc in range(8):
                            P.mm(op_[:], za[:, kc, t * 128:(t + 1) * 128], Wo[:, kc, half * 512:(half + 1) * 512], kc == 0, kc == 7,
                                 [Wok, ("za", kc)], [opk], inc=(kc == 7))
                        P.act(junk16[:, 0:512], op_[:], AF.Square, [opk], ["junk16", ("ssB", half)], accum=small[:, 8 + half:9 + half])
                    P.tt("vector", small[:, 10:11], small[:, 8:9], small[:, 9:10], ALU.add, [("ssB", 0), ("ssB", 1)], ["ssB2"])
                    P.act(small[:, 11:12], small[:, 10:11], AF.Ln, ["ssB2"], ["sqB"], scale=1.0 / 1024, bias=EPS)
                    P.act(small[:, 12:13], small[:, 11:12], AF.Exp, ["sqB"], ["rstdB"], scale=-0.5)
                    xin = xt[tile_i % 2]
                    xink = "xt%d" % (tile_i % 2)
                    P.dma("sync", xin[:], xs[tile_i * 128:(tile_i + 1) * 128, :], (), [xink], xink)
                    for half in range(2):
                        op_, opk = ops_[half]
                        hs_ = slice(half * 512, (half + 1) * 512)
                        P.stt(xb[:, hs_], op_[:], small[:, 12:13], gvec1[:, hs_], ALU.mult, ALU.mult, [opk, "rstdB", "gvec1"], [xbk])
                    P.tt("vector", xb[:], xb[:], xin[:], ALU.add, [xbk, xink], [xbk])
                    P.dma("sync", out[tile_i * 128:(tile_i + 1) * 128, :], xb[:], [xbk], [("out", tile_i)], "o%d" % (tile_i % 2))
                    return norm_pre(None, xb, xbk)

                def stB(t, xn, j=j):
                    tile_i = j * 4 + t
                    norm_post(xn, 4, (hxT[:, :, tile_i * 128:(tile_i + 1) * 128], None), [("hx2", tile_i), ("hxT", j)], tbB, "tbB")

                def stC(t, j=j):
                    tile_i = j * 4 + t
                    lp, lpk = nextpb()
                    for kc in range(8):
                        P.mm(lp[:, 0:64], hxT[:, kc, tile_i * 128:(tile_i + 1) * 128], wrt16[:, kc, :], kc == 0, kc == 7,
                             [("hx2", tile_i), "wrt16"], [lpk], inc=(kc == 7))
                    sc_, sel_, eq_, t64 = tk[:, 0, :], tk[:, 1, :], tk[:, 2, :], tk[:, 3, :]
                    m1, m2, gs_, top8, gm_, pen = tk[:, 4, 0:8], tk[:, 4, 8:16], tk[:, 4, 16:24], tk[:, 4, 24:32], tk[:, 4, 32:40], tk[:, 4, 40:48]
                    K = ["tk"]
                    P.act(sc_, lp[:, 0:64], AF.Sigmoid, [lpk], K)
                    P.tt("vector", sel_, sc_, rbias_sb[:], ALU.add, K + ["rbias"], K)
                    s3 = sel_.rearrange("p (g e) -> p g e", e=8)
                    P.op("vector", lambda e, s3=s3, m1=m1: e.tensor_reduce(out=m1, in_=s3, axis=AX.X, op=ALU.max), K, K)
                    P.tt("vector", eq_.rearrange("p (g e) -> p g e", e=8), s3, m1.unsqueeze(2).to_broadcast([128, 8, 8]), ALU.is_equal, K, K)
                    P.stt(t64, eq_, -1e30, sel_, ALU.mult, ALU.add, K, K)
                    P.op("vector", lambda e, t64=t64, m2=m2: e.tensor_reduce(out=m2, in_=t64.rearrange("p (g e) -> p g e", e=8), axis=AX.X, op=ALU.max), K, K)
                    P.tt("vector", gs_, m1, m2, ALU.add, K, K)
                    P.op("vector", lambda e, gs_=gs_, top8=top8: e.max(out=top8, in_=gs_), K, K)
                    P.ts("vector", gm_, gs_, top8[:, 3:4], None, ALU.is_ge, None, K, K)
                    P.ts("vector", pen, gm_, 1e30, -1e30, ALU.mult, ALU.add, K, K)
                    P.tt("vector", t64.rearrange("p (g e) -> p g e", e=8), s3, pen.unsqueeze(2).to_broadcast([128, 8, 8]), ALU.add, K, K)
                    P.op("vector", lambda e, t64=t64, top8=top8: e.max(out=top8, in_=t64), K, K)
                    P.ts("vector", eq_, t64, top8[:, 7:8], None, ALU.is_ge, None, K, K)
                    P.tt("vector", eq_, eq_, sc_, ALU.mult, K, K)
                    P.op("vector", lambda e, eq_=eq_, m1=m1: e.tensor_reduce(out=m1[:, 0:1], in_=eq_, axis=AX.X, op=ALU.add), K, K)
                    P.op("vector", lambda e, m1=m1: e.reciprocal(out=m1[:, 1:2], in_=m1[:, 0:1]), K, K)
                    P.ts("vector", tk[:, 5 + (tile_i % 2), :], eq_, m1[:, 1:2], 2.5, ALU.mult, ALU.mult, K, [("gate", tile_i % 2)])

                def stD(t, j=j):
                    tile_i = j * 4 + t
                    gp_, gpk = nextpb()
                    P.tp(gp_[0:64, 0:128], tk[:, 5 + (tile_i % 2), :], identf[:], [("gate", tile_i % 2), "identf"], [gpk], inc=True)
                    P.act(gT[0:64, tile_i * 128:(tile_i + 1) * 128], gp_[0:64, 0:128], AF.Copy, [gpk], ["gT"])

                def tail_gen(stA=stA, stB=stB, stC=stC, stD=stD):
                    xns = {}
                    for step in range(4 + 3):
                        if step < 4:
                            xns[step] = stA(step)
                            yield
                        if 0 <= step - 1 < 4:
                            stB(step - 1, xns[step - 1])
                            yield
                        if 0 <= step - 2 < 4:
                            stC(step - 2)
                            yield
                        if 0 <= step - 3 < 4:
                            stD(step - 3)
                            yield
                pending_tail[0] = tail_gen()
            for _ in pending_tail[0]:
                pass
            P.dma("sync", gscr[0:65, :], gT[0:65, :], ["gT"], ["gscr"], "gsw")
            if dbg:
                P.op("sync", lambda e: None, ["gscr", "dbgo"] + [("out", i) for i in range(16)], (), inc=False)
                P.pend["sync"] = []
            P.run()
        if stop == "B":
            return nc

        with ExitStack() as ec:
            NE = 4
            ering = [sb(ec, "E%d" % i, [128, 6144], BF16) for i in range(NE)]
            acc = sb(ec, "acc", [128, 16, 1024], F32)
            hbuf = [[sb(ec, "h%d%d" % (p_, e_), [128, 2, 512], BF16) for e_ in range(2)] for p_ in range(2)]
            gbc = [sb(ec, "gbc%d" % i, [128, 512], BF16) for i in range(4)]
            s_sb = [sb(ec, "ssb%d" % i, [128, 512], BF16) for i in range(3)]
            su_sb = [sb(ec, "susb%d" % i, [128, 512], BF16) for i in range(3)]
            fo = [sb(ec, "fo%d" % i, [128, 1024], F32) for i in range(2)]
            gu = [ps(ec, "gu%d" % i, [128, 512], F32) for i in range(4)]
            ob = [ps(ec, "ob%d" % i, [128, 512], F32) for i in range(4)]

            def eload(e):
                s = e % NE
                P.dma("gpsimd", ering[s][:, 0:2048].rearrange("p (k n) -> p k n", n=256), wg[e].rearrange("(k p) n -> p k n", p=128), (), ["Eg%d" % s], "Eg%d" % s)
                P.dma("gpsimd", ering[s][:, 2048:4096].rearrange("p (k n) -> p k n", n=256), wu[e].rearrange("(k p) n -> p k n", p=128), (), ["Eu%d" % s], "Eu%d" % s)
                P.dma("gpsimd", ering[s][:, 4096:6144].rearrange("p (k n) -> p k n", n=1024), wd[e].rearrange("(k p) n -> p k n", p=128), (), ["Ed%d" % s], "Ed%d" % s)

            groups = [(2 * g, 2 * g + 1) for g in range(32)] + [(64,)]
            for e in groups[0] + groups[1]:
                eload(e)
            gbi = [0]
            gui = [0]
            si = [0]
            obi = [0]
            pend_d = [None]

            def do_down(gi_, grp, b, par):
                for t in range(4):
                    tile_i = b * 4 + t
                    for half in range(2):
                        o_i = obi[0] % 4
                        obi[0] += 1
                        op_, opk = ob[o_i], "ob%d" % o_i
                        n = len(grp) * 2
                        i = 0
                        for ei, e in enumerate(grp):
                            s = e % NE
                            wdv = ering[s][:, 4096:6144].rearrange("p (k n) -> p k n", n=1024)
                            for c in range(2):
                                P.mm(op_[:], hbuf[par][ei][:, c, t * 128:(t + 1) * 128], wdv[:, c, half * 512:(half + 1) * 512],
                                     i == 0, i == n - 1, ["Ed%d" % s, ("h", par, ei)], [opk], inc=(i == n - 1))
                                i += 1
                        dst = acc[:, tile_i, half * 512:(half + 1) * 512]
                        ak = ("acc", tile_i)
                        if gi_ == 0:
                            P.copy("vector", dst, op_[:], [opk], [ak])
                        else:
                            P.tt("vector", dst, dst, op_[:], ALU.add, [opk, ak], [ak])

            steps = [(gi_, grp, b) for gi_, grp in enumerate(groups) for b in range(4)]
            gbmap = {}

            def gbc_load(si_):
                gi_, grp, b = steps[si_]
                for ei, e in enumerate(grp):
                    gb_i = gbi[0] % 4
                    gbi[0] += 1
                    gk = "gbc%d" % gb_i
                    gbmap[(si_, ei)] = (gb_i, gk)
                    P.dma("gpsimd", gbc[gb_i][:].unsqueeze(1), gscr[e:e + 1, b * 512:(b + 1) * 512].partition_broadcast(128),
                          ["gscr"], [gk], gk)
            gbc_load(0)
            for si_, (gi_, grp, b) in enumerate(steps):
                par = si_ % 2
                blk = slice(b * 512, (b + 1) * 512)
                for ei, e in enumerate(grp):
                    s = e % NE
                    gb_i, gk = gbmap[(si_, ei)]
                    wgv = ering[s][:, 0:2048].rearrange("p (k n) -> p k n", n=256)
                    wuv = ering[s][:, 2048:4096].rearrange("p (k n) -> p k n", n=256)
                    for c in range(2):
                        g_i = gui[0] % 2
                        gui[0] += 1
                        gp_, gpk = gu[2 * g_i], "gu%d" % (2 * g_i)
                        up_, upk = gu[2 * g_i + 1], "gu%d" % (2 * g_i + 1)
                        for (pp, ppk, wv, wk_) in ((gp_, gpk, wgv, "Eg%d" % s), (up_, upk, wuv, "Eu%d" % s)):
                            for kc in range(8):
                                P.mm(pp[:], wv[:, kc, c * 128:(c + 1) * 128], hxT[:, kc, blk], kc == 0, kc == 7,
                                     [wk_] + [("hx2", b * 4 + t_) for t_ in range(4)], [ppk], inc=(kc == 7))
                        s_i = si[0] % 3
                        si[0] += 1
                        sk, suk = "ssb%d" % s_i, "susb%d" % s_i
                        P.act(s_sb[s_i][:], gp_[:], AF.Silu, [gpk], [sk])
                        P.tt("vector", su_sb[s_i][:], up_[:], s_sb[s_i][:], ALU.mult, [upk, sk], [suk])
                        P.tt("vector", hbuf[par][ei][:, c, :], su_sb[s_i][:], gbc[gb_i][:], ALU.mult, [suk, gk], [("h", par, ei)])
                if si_ + 1 < len(steps):
                    gbc_load(si_ + 1)
                if pend_d[0] is not None:
                    do_down(*pend_d[0])
                    pg = pend_d[0][0]
                    if pend_d[0][2] == 3 and pg + 2 < len(groups):
                        for e in groups[pg + 2]:
                            eload(e)
                pend_d[0] = (gi_, grp, b, par)
            do_down(*pend_d[0])
            for tile_i in range(16):
                xin = xt[tile_i % 2]
                xink = "xt%d" % (tile_i % 2)
                P.dma("sync", xin[:], out[tile_i * 128:(tile_i + 1) * 128, :], [("out", tile_i)], [xink], xink)
                ak = ("acc", tile_i)
                P.act(junk16[:], acc[:, tile_i, :], AF.Square, [ak], ["junk16", "ssC"], accum=small[:, 16:17])
                P.act(small[:, 17:18], small[:, 16:17], AF.Sqrt, ["ssC"], ["sqC"], scale=1.0 / 1024, bias=EPS)
                P.op("vector", lambda e: e.reciprocal(out=small[:, 18:19], in_=small[:, 17:18]), ["sqC"], ["rstdC"])
                f_ = fo[tile_i % 2]
                fk = "fo%d" % (tile_i % 2)
                P.stt(f_[:], acc[:, tile_i, :], small[:, 18:19], gvec2[:], ALU.mult, ALU.mult, [ak, "rstdC", "gvec2"], [fk])
                P.tt("vector", f_[:], f_[:], xin[:], ALU.add, [fk, xink], [fk])
                P.dma("sync", out[tile_i * 128:(tile_i + 1) * 128, :], f_[:], [fk], [("out", tile_i)], "o%d" % (tile_i % 2))
            P.op("sync", lambda e: None, [("out", i) for i in range(16)], (), inc=False)
            P.pend["sync"] = []
            P.run()
    return nc


def _prep_inputs(I, cid):
    b = cid // 2
    odd = cid % 2 == 1
    f = np.float32
    x = I['x'][b]
    ctx = I['ctx'][b]
    if odd:
        x = x[::-1]
        ctx = ctx[::-1]
    dP, dQ = (1, 0) if odd else (0, 1)
    w_sc = I['w_sc'][0]
    w_dw = I['w_dw'][0]
    bm = I['b_mod'][0].reshape(6, 1024)
    R = {
        'pre1_g': I['pre1_g'][0], 'pre2_g': I['pre2_g'][0], 'b_dw': I['b_dw'][0],
        'ln_g': I['ln_conv_g'][0], 'ln_b': I['ln_conv_b'][0],
        'bscP': I['b_sc'][0, dP], 'bscQ': I['b_sc'][0, dQ],
        'braP': I['b_rg_a'][0, dP], 'braQ': I['b_rg_a'][0, dQ],
        'brxP': I['b_rg_x'][0, dP], 'brxQ': I['b_rg_x'][0, dQ],
        'lamP': I['lru_lambda'][0, dP], 'lamQ': I['lru_lambda'][0, dQ],
        'bm_sh1': bm[0], 'bm_sc1': bm[1], 'bm_sh2': bm[3], 'bm_sc2': bm[4],
    }
    for i in range(31):
        R['dw%d' % i] = w_dw[30 - i] if odd else w_dw[i]
    for i in range(4):
        R['scP%d' % i] = w_sc[dP][3 - i] if odd else w_sc[dP][i]
        R['scQ%d' % i] = w_sc[dQ][3 - i] if odd else w_sc[dQ][i]
    V = np.stack([R[n] for n in VROWS]).astype(f)
    vecs = np.ascontiguousarray(V.reshape(len(VROWS), 8, 128).transpose(2, 0, 1).reshape(128, NV))
    cvT = np.ascontiguousarray(np.concatenate([I['c'][b].reshape(8, 128).T, I['c_ctx'].reshape(8, 128).T], axis=1)).astype(f)
    rows = np.stack([I['post1_g'][0], bm[2], I['post2_g'][0], bm[5]]).astype(f)
    w_rg = np.concatenate([I['w_rg_a'][0, dP], I['w_rg_x'][0, dP], I['w_rg_a'][0, dQ], I['w_rg_x'][0, dQ]], axis=0)
    return {
        'xs': np.ascontiguousarray(x, f), 'ctxs': np.ascontiguousarray(ctx, f), 'cvT': cvT, 'vecs': vecs, 'rows': rows,
        'rbias': np.ascontiguousarray(I['router_bias'][0].reshape(1, 64), f),
        'ident': np.eye(128, dtype=f),
        'w_rg': np.ascontiguousarray(w_rg, f),
    }


def kernel(**inputs):
    I = {k: np.asarray(v) for k, v in inputs.items()}
    shared = {
        'w_mod': np.ascontiguousarray(I['w_mod'][0], np.float32),
        'w_in': np.ascontiguousarray(I['w_in'][0], np.float32),
        'w_co': np.ascontiguousarray(I['w_conv_out'][0], np.float32),
        'w_ro': np.ascontiguousarray(I['w_rnn_out'][0], np.float32),
        'w_o': np.ascontiguousarray(I['w_out'][0], np.float32),
        'w_rt': np.ascontiguousarray(I['w_router'][0], np.float32),
        'wg': np.ascontiguousarray(np.concatenate([I['w_e_gate'][0], I['w_s_gate']], axis=0), np.float32),
        'wu': np.ascontiguousarray(np.concatenate([I['w_e_up'][0], I['w_s_up']], axis=0), np.float32),
        'wd': np.ascontiguousarray(np.concatenate([I['w_e_down'][0], I['w_s_down']], axis=0), np.float32),
    }
    in_maps = []
    for cid in range(8):
        m = dict(shared)
        m.update(_prep_inputs(I, cid))
        in_maps.append(m)
    nc = build()
    res = run_bass_kernel_spmd(nc, in_maps, core_ids=list(range(8)))
    outp = np.empty((4, 4096, 1024), np.float32)
    for cid in range(8):
        o = res.results[cid]["out"]
        b = cid // 2
        if cid % 2 == 0:
            outp[b, 0:2048] = o
        else:
            outp[b, 2048:4096] = o[::-1]
    return outp
```
